# Optimizing a Trainium2 kernel written in Bass

```python
import math
import jax, jax.numpy as jnp
from jax import lax
import numpy as np

D_MODEL = 1024
BATCH = 4
SEQ = 8192
DEPTH = 2

CHUNK = 128
EPS = 1e-6
SSD_HEADS = 16
SSD_HEAD_DIM = 64
SSD_WIDTH = SSD_HEADS * SSD_HEAD_DIM
SSD_GROUPS = 2
SSD_STATE = 128
CONV_WIDTH = 4
SSD_CONV_CH = SSD_WIDTH + 2 * SSD_GROUPS * SSD_STATE
SSD_IN = 2 * SSD_WIDTH + 2 * SSD_GROUPS * SSD_STATE + SSD_HEADS
S5_GROUP_CH = 16
S5_GROUPS = 32
S5_WIDTH = S5_GROUPS * S5_GROUP_CH
S5_STATE = 64
RET_HEADS = 8
RET_KEY_DIM = 32
RET_VAL_DIM = 64
RET_QK = RET_HEADS * RET_KEY_DIM
RET_WIDTH = RET_HEADS * RET_VAL_DIM
ROPE_BASE = 10000.0
MIX_WIDTH = SSD_WIDTH + S5_WIDTH + RET_WIDTH
IN_WIDTH = SSD_IN + 2 * S5_WIDTH + 2 * RET_QK + 2 * RET_WIDTH
SPLITS = [int(v) for v in np.cumsum([SSD_IN, S5_WIDTH, S5_WIDTH, RET_QK, RET_QK, RET_WIDTH])]

kernel_name = "hybrid_ssd_s5_retention_parallel_heads"


def _rmsnorm(x, w):
    xf = x.astype(jnp.float32)
    return xf * lax.rsqrt(jnp.mean(xf * xf, axis=-1, keepdims=True) + EPS) * w


def _causal_dwconv(x, w, b):
    y = lax.conv_general_dilated(
        x, w.astype(x.dtype)[:, None, :], window_strides=(1,),
        padding=[(CONV_WIDTH - 1, 0)], dimension_numbers=("NWC", "WIO", "NWC"),
        feature_group_count=x.shape[-1])
    return y + b


def _ssd_branch(p, conv_w, conv_b, dt_bias, a_log, d_skip, norm_w):
    b, l, _ = p.shape
    nc = l // CHUNK
    hg = SSD_HEADS // SSD_GROUPS
    z, xbc, dt = jnp.split(p, [SSD_WIDTH, SSD_WIDTH + SSD_CONV_CH], axis=-1)
    xbc = jax.nn.silu(_causal_dwconv(xbc, conv_w, conv_b))
    xs, bm, cm = jnp.split(xbc, [SSD_WIDTH, SSD_WIDTH + SSD_GROUPS * SSD_STATE], axis=-1)
    xs = xs.reshape(b, nc, CHUNK, SSD_GROUPS, hg, SSD_HEAD_DIM)
    bm = bm.reshape(b, nc, CHUNK, SSD_GROUPS, SSD_STATE)
    cm = cm.reshape(b, nc, CHUNK, SSD_GROUPS, SSD_STATE)
    dt = jax.nn.softplus(dt + dt_bias).reshape(b, nc, CHUNK, SSD_GROUPS, hg)
    a = -jnp.exp(a_log.astype(jnp.float32)).reshape(SSD_GROUPS, hg)
    xdt = xs * dt[..., None]
    acum = jnp.cumsum((dt * a).transpose(0, 3, 4, 1, 2), axis=-1)
    seg = acum[..., :, None] - acum[..., None, :]
    causal = jnp.tril(jnp.ones((CHUNK, CHUNK), dtype=bool))
    decay = jnp.exp(jnp.where(causal, seg, -jnp.inf))
    cb = jnp.einsum("bclgn,bcsgn->bgcls", cm, bm)
    y_diag = jnp.einsum("bgrcls,bcsgrp->bclgrp", cb[:, :, None] * decay, xdt)
    decay_states = jnp.exp(acum[..., -1:] - acum)
    states = jnp.einsum("bclgn,bgrcl,bclgrp->bcgrpn", bm, decay_states, xdt)
    chunk_decay = jnp.exp(acum[..., -1])

    def step(carry, inp):
        s_c, d_c = inp
        return carry * d_c[..., None, None] + s_c, carry

    init = jnp.zeros(states.shape[:1] + states.shape[2:], states.dtype)
    _, prev = lax.scan(step, init, (states.transpose(1, 0, 2, 3, 4, 5),
                                     chunk_decay.transpose(3, 0, 1, 2)))
    y_off = jnp.einsum("bclgn,cbgrpn->bclgrp", cm, prev) * \
        jnp.exp(acum).transpose(0, 3, 4, 1, 2)[..., None]
    y = y_diag + y_off + xs * d_skip.reshape(SSD_GROUPS, hg)[:, :, None]
    y = y.reshape(b, l, SSD_WIDTH) * jax.nn.silu(z)
    y = y.reshape(b, l, SSD_GROUPS, SSD_WIDTH // SSD_GROUPS)
    y = y * lax.rsqrt(jnp.mean(y * y, axis=-1, keepdims=True) + EPS)
    return y.reshape(b, l, SSD_WIDTH) * norm_w


def _s5_branch(u, lam_re, lam_im, b_re, b_im, c_re, c_im, d_skip, log_step, w_glu, b_glu):
    f32 = jnp.float32
    b, l, _ = u.shape
    lam_re = lam_re.astype(f32); lam_im = lam_im.astype(f32)
    step = jnp.exp(log_step.astype(f32))[:, None]
    mag = jnp.exp(lam_re * step)
    ang = lam_im * step
    lb_re = mag * jnp.cos(ang)
    lb_im = mag * jnp.sin(ang)
    den = lam_re * lam_re + lam_im * lam_im
    f_re = ((lb_re - 1.0) * lam_re + lb_im * lam_im) / den
    f_im = (lb_im * lam_re - (lb_re - 1.0) * lam_im) / den
    bb_re = f_re[..., None] * b_re - f_im[..., None] * b_im
    bb_im = f_re[..., None] * b_im + f_im[..., None] * b_re
    ug = u.reshape(b, l, S5_GROUPS, S5_GROUP_CH)
    bu_re = jnp.einsum("blgc,gpc->blgp", ug, bb_re)
    bu_im = jnp.einsum("blgc,gpc->blgp", ug, bb_im)
    a_re = jnp.broadcast_to(lb_re, (1, l, S5_GROUPS, S5_STATE))
    a_im = jnp.broadcast_to(lb_im, (1, l, S5_GROUPS, S5_STATE))

    def combine(ei, ej):
        ar_i, ai_i, br_i, bi_i = ei
        ar_j, ai_j, br_j, bi_j = ej
        return (ar_j * ar_i - ai_j * ai_i,
                ar_j * ai_i + ai_j * ar_i,
                ar_j * br_i - ai_j * bi_i + br_j,
                ar_j * bi_i + ai_j * br_i + bi_j)

    _, _, s_re, s_im = lax.associative_scan(combine, (a_re, a_im, bu_re, bu_im), axis=1)
    y = jnp.einsum("blgp,gcp->blgc", s_re, c_re) - jnp.einsum("blgp,gcp->blgc", s_im, c_im)
    y = y.reshape(b, l, S5_WIDTH) + d_skip * u
    y = jax.nn.gelu(y)
    return y * jax.nn.sigmoid(y @ w_glu + b_glu)


def _rotary(t, cos, sin):
    t1, t2 = jnp.split(t, 2, axis=-1)
    c = cos[:, None, :]
    s = sin[:, None, :]
    return jnp.concatenate([t1 * c - t2 * s, t1 * s + t2 * c], axis=-1)


def _retention_branch(q, k, v, norm_w):
    b, l, _ = q.shape
    nc = l // CHUNK
    pos = jnp.arange(l, dtype=jnp.float32)
    inv_freq = ROPE_BASE ** (-jnp.arange(0, RET_KEY_DIM, 2, dtype=jnp.float32) / RET_KEY_DIM)
    ang = pos[:, None] * inv_freq[None, :]
    cos, sin = jnp.cos(ang), jnp.sin(ang)
    q = _rotary(q.reshape(b, l, RET_HEADS, RET_KEY_DIM), cos, sin)
    k = _rotary(k.reshape(b, l, RET_HEADS, RET_KEY_DIM), cos, sin) * (RET_KEY_DIM ** -0.5)
    v = v.reshape(b, l, RET_HEADS, RET_VAL_DIM)
    log_g = jnp.log1p(-jnp.exp2(-5.0 - jnp.arange(RET_HEADS, dtype=jnp.float32)))
    q = q.reshape(b, nc, CHUNK, RET_HEADS, RET_KEY_DIM)
    k = k.reshape(b, nc, CHUNK, RET_HEADS, RET_KEY_DIM)
    v = v.reshape(b, nc, CHUNK, RET_HEADS, RET_VAL_DIM)
    idx = jnp.arange(CHUNK, dtype=jnp.float32)
    diff = idx[:, None] - idx[None, :]
    dmat = jnp.where(diff >= 0, jnp.exp(jnp.maximum(diff, 0.0) * log_g[:, None, None]), 0.0)
    scores = jnp.einsum("bcthd,bcshd->bchts", q, k) * dmat
    y_in = jnp.einsum("bchts,bcshe->bcthe", scores, v)
    k_dec = k * jnp.exp((CHUNK - 1.0 - idx)[:, None] * log_g[None, :])[..., None]
    kv = jnp.einsum("bcthd,bcthe->bchde", k_dec, v)
    chunk_decay = jnp.exp(CHUNK * log_g)

    def step(carry, kv_c):
        return carry * chunk_decay[:, None, None] + kv_c, carry

    init = jnp.zeros((b, RET_HEADS, RET_KEY_DIM, RET_VAL_DIM), kv.dtype)
    _, prev = lax.scan(step, init, kv.transpose(1, 0, 2, 3, 4))
    q_dec = q * jnp.exp((idx + 1.0)[:, None] * log_g[None, :])[..., None]
    y_cross = jnp.einsum("bcthd,cbhde->bcthe", q_dec, prev)
    y = (y_in + y_cross).reshape(b, l, RET_HEADS, RET_VAL_DIM)
    y = y * lax.rsqrt(jnp.mean(y * y, axis=-1, keepdims=True) + EPS)
    return y.reshape(b, l, RET_WIDTH) * norm_w


def setup_inputs(seed: int = 0) -> dict:
    key = jax.random.key(seed)
    ks = jax.random.split(key, 24)
    f32 = jnp.float32
    nrm = lambda k, s, sc: jax.random.normal(k, s, f32) * sc
    lo, hi = math.log(1e-3), math.log(1e-1)
    dt0 = jnp.exp(jax.random.uniform(ks[5], (DEPTH, SSD_HEADS), f32) * (hi - lo) + lo)
    inv_sqrt2 = 1.0 / math.sqrt(2.0)
    return {
        "x": nrm(ks[0], (BATCH, SEQ, D_MODEL), 1.0),
        "norm_w": 1.0 + nrm(ks[1], (DEPTH, D_MODEL), 0.02),
        "w_in": nrm(ks[2], (DEPTH, D_MODEL, IN_WIDTH), D_MODEL ** -0.5),
        "conv_w": nrm(ks[3], (DEPTH, CONV_WIDTH, SSD_CONV_CH), CONV_WIDTH ** -0.5),
        "conv_b": nrm(ks[4], (DEPTH, SSD_CONV_CH), 0.02),
        "dt_bias": dt0 + jnp.log(-jnp.expm1(-dt0)),
        "a_log": jnp.log(jax.random.uniform(ks[6], (DEPTH, SSD_HEADS), f32, 1.0, 16.0)),
        "d_ssd": 1.0 + nrm(ks[7], (DEPTH, SSD_HEADS), 0.02),
        "ssd_norm_w": 1.0 + nrm(ks[8], (DEPTH, SSD_WIDTH), 0.02),
        "s5_lambda_re": -0.5 + nrm(ks[9], (DEPTH, S5_GROUPS, S5_STATE), 0.01),
        "s5_lambda_im": math.pi * jnp.arange(S5_STATE, dtype=f32) + nrm(ks[10], (DEPTH, S5_GROUPS, S5_STATE), 0.01),
        "s5_b_re": nrm(ks[11], (DEPTH, S5_GROUPS, S5_STATE, S5_GROUP_CH), S5_GROUP_CH ** -0.5 * inv_sqrt2),
        "s5_b_im": nrm(ks[12], (DEPTH, S5_GROUPS, S5_STATE, S5_GROUP_CH), S5_GROUP_CH ** -0.5 * inv_sqrt2),
        "s5_c_re": nrm(ks[13], (DEPTH, S5_GROUPS, S5_GROUP_CH, S5_STATE), S5_STATE ** -0.5 * inv_sqrt2),
        "s5_c_im": nrm(ks[14], (DEPTH, S5_GROUPS, S5_GROUP_CH, S5_STATE), S5_STATE ** -0.5 * inv_sqrt2),
        "s5_d": nrm(ks[15], (DEPTH, S5_WIDTH), 1.0),
        "s5_log_step": jax.random.uniform(ks[16], (DEPTH, S5_GROUPS), f32) * (hi - lo) + lo,
        "s5_w_glu": nrm(ks[17], (DEPTH, S5_WIDTH, S5_WIDTH), S5_WIDTH ** -0.5),
        "s5_b_glu": nrm(ks[18], (DEPTH, S5_WIDTH), 0.02),
        "ret_norm_w": 1.0 + nrm(ks[19], (DEPTH, RET_WIDTH), 0.02),
        "w_out": nrm(ks[20], (DEPTH, MIX_WIDTH, D_MODEL), MIX_WIDTH ** -0.5),
        "final_norm_w": 1.0 + nrm(ks[21], (D_MODEL,), 0.02),
    }


def reference(x, norm_w, w_in, conv_w, conv_b, dt_bias, a_log, d_ssd, ssd_norm_w,
              s5_lambda_re, s5_lambda_im, s5_b_re, s5_b_im, s5_c_re, s5_c_im, s5_d,
              s5_log_step, s5_w_glu, s5_b_glu, ret_norm_w, w_out, final_norm_w):
    out_dtype = x.dtype
    h_res = x.astype(jnp.float32)
    for i in range(DEPTH):
        h = _rmsnorm(h_res, norm_w[i])
        proj = h @ w_in[i]
        p_ssd, s5_gate, s5_u, r_q, r_k, r_v, r_gate = jnp.split(proj, SPLITS, axis=-1)
        y_ssd = _ssd_branch(p_ssd, conv_w[i], conv_b[i], dt_bias[i], a_log[i], d_ssd[i], ssd_norm_w[i])
        y_s5 = _s5_branch(s5_u, s5_lambda_re[i], s5_lambda_im[i], s5_b_re[i], s5_b_im[i],
                          s5_c_re[i], s5_c_im[i], s5_d[i], s5_log_step[i], s5_w_glu[i],
                          s5_b_glu[i]) * jax.nn.silu(s5_gate)
        y_ret = _retention_branch(r_q, r_k, r_v, ret_norm_w[i]) * jax.nn.silu(r_gate)
        y = jnp.concatenate([y_ssd, y_s5, y_ret], axis=-1)
        h_res = h_res + y @ w_out[i]
    return _rmsnorm(h_res, final_norm_w).astype(out_dtype)
```

```python
import math
import numpy as np
from contextlib import ExitStack
import concourse.bass as bass
import concourse.mybir as mybir
from concourse.bass_utils import run_bass_kernel_spmd

F32 = mybir.dt.float32
BF16 = mybir.dt.bfloat16
AF = mybir.ActivationFunctionType
ALU = mybir.AluOpType

P = 128
T = 128
NCH = 2
SP = T * NCH
J = 4
NB = SP // J
DM = 1024
EPS = 1e-6
NG = 21
SEQ = 8192
TWO_PI = 2.0 * math.pi

FM_ORIG = ([0 + 128 * i for i in range(8)] + [1024 + 128 * i for i in range(12)] +
           [2576 + 128 * i for i in range(4)] + [3088 + 128 * i for i in range(4)] +
           [3600, 3728] + [3856, 3984] + [4624 + 128 * i for i in range(4)])
assert len(FM_ORIG) == 36

_pp_fields = [("normw", 8), ("convw", 48), ("convb", 12), ("dtb", 16), ("alog", 16), ("dssd", 16),
              ("ssdnw", 8), ("s5d", 4), ("bglu", 4), ("retnw", 4), ("lamre", 16), ("lamim", 16),
              ("lstep", 16)]
_pb_fields = [("bre", 512), ("bim", 512), ("cre", 512), ("cim", 512)]
OFF = {}
_o = 0
for _n, _w in _pp_fields:
    OFF[_n] = _o
    _o += _w
NPP = _o
OFFB = {}
_o = 0
for _n, _w in _pb_fields:
    OFFB[_n] = _o
    _o += _w
NPB = _o

_c32_fields = [("U", 128), ("ones", 128), ("tri", 128), ("dmat", 1024), ("qdec", 256), ("kdec", 8),
               ("gch", 2), ("cos", 128), ("sin", 128), ("cc", 64), ("sc", 64), ("rmask", 4)]
C32 = {}
_o = 0
for _n, _w in _c32_fields:
    C32[_n] = _o
    _o += _w
NC32 = _o


class AS:
    def __init__(self, nc, es):
        self.nc = nc
        self.es = es
        self.eng = {"pe": nc.tensor, "act": nc.scalar, "dve": nc.vector, "pool": nc.gpsimd, "sp": nc.sync}
        self.sem = {k: es.enter_context(nc.semaphore("sem_" + k)) for k in self.eng}
        self.cnt = {k: 0 for k in self.eng}
        self.waited = {k: {} for k in self.eng}
        self.lastw = {}
        self.readers = {}
        self.dsem = {}
        self.nins = 0

    def _need(self, e, deps):
        best = {}
        for (src, val) in deps:
            if self.waited[e].get(src, 0) >= val:
                continue
            if best.get(src, 0) < val:
                best[src] = val
        for src, val in best.items():
            sem = self.dsem[src][0] if src in self.dsem else self.sem[src]
            self.eng[e].wait_ge(sem, val)
            self.waited[e][src] = val
            self.nins += 1

    def _deps(self, reads, writes):
        deps = []
        for k in reads:
            w = self.lastw.get(k)
            if w is not None:
                deps.append(w)
        for k in writes:
            w = self.lastw.get(k)
            if w is not None:
                deps.append(w)
            deps.extend(self.readers.get(k, ()))
        return deps

    def _commit(self, tag, reads, writes):
        for k in reads:
            lst = self.readers.setdefault(k, [])
            lst[:] = [t for t in lst if t[0] != tag[0]]
            lst.append(tag)
        for k in writes:
            self.lastw[k] = tag
            self.readers[k] = []

    def op(self, e, fn, reads=(), writes=()):
        self._need(e, self._deps(reads, writes))
        ins = fn(self.eng[e])
        self.cnt[e] += 1
        ins.then_inc(self.sem[e], 1)
        self.nins += 1
        self._commit((e, self.cnt[e]), reads, writes)

    def group(self, e, fns, reads=(), writes=()):
        self._need(e, self._deps(reads, writes))
        ins = None
        for fn in fns:
            ins = fn(self.eng[e])
            self.nins += 1
        self.cnt[e] += 1
        ins.then_inc(self.sem[e], 1)
        self._commit((e, self.cnt[e]), reads, writes)

    def dma(self, q, chan, out, in_, reads=(), writes=(), **kw):
        if chan not in self.dsem:
            self.dsem[chan] = [self.es.enter_context(self.nc.semaphore("dsem_" + chan)), 0]
        self._need(q, self._deps(reads, writes))
        if q == "pool":
            kw.setdefault("max_dma_last_dim", 2048)
        ins = self.eng[q].dma_start(out=out, in_=in_, **kw)
        self.dsem[chan][1] += 16
        ins.then_inc(self.dsem[chan][0], 16)
        self.nins += 1
        self._commit((chan, self.dsem[chan][1]), reads, writes)

    def barrier(self):
        for e in self.eng:
            deps = [(s_, self.cnt[s_]) for s_ in self.eng if s_ != e and self.cnt[s_] > 0]
            deps += [(ch, v[1]) for ch, v in self.dsem.items() if v[1] > 0]
            self._need(e, deps)

    def finish(self, e="sp"):
        deps = list(self.lastw.values())
        for l in self.readers.values():
            deps.extend(l)
        self._need(e, deps)


import os
VAR_RMAX = int(os.environ.get("K_RMAX", "4"))
VAR_SKIPROT = int(os.environ.get("K_SKIPROT", "0"))


class _Stop(Exception):
    pass


def build_program(nspan=SEQ // SP, nlayer=2, debug=False, stage=None):
    nc = bass.Bass("TRN2", target_bir_lowering=False)

    def stg(k):
        if stage == k:
            raise _Stop()
    L = nspan * SP
    dram = lambda n, s, d, k: nc.dram_tensor(n, s, d, kind=k).ap()
    x_d = dram("x", [L, DM], F32, "ExternalInput")
    win_d = dram("win_g", [2, NG, P, 8, 256], F32, "ExternalInput")
    wout_d = dram("wout_h", [2, P, 16, DM], F32, "ExternalInput")
    wglu_d = dram("wglu_h", [2, P, 4, 512], F32, "ExternalInput")
    pp_d = dram("pp", [2, P, NPP], F32, "ExternalInput")
    pb_d = dram("pb", [2, P, NPB], F32, "ExternalInput")
    cbrow_d = dram("cbrow", [2, 1, 1280], F32, "ExternalInput")
    fnw_d = dram("fnw", [1, DM], F32, "ExternalInput")
    c32_d = dram("cst32", [P, NC32], F32, "ExternalInput")
    cbf_d = dram("cstbf", [P, 512], F32, "ExternalInput")
    out_d = dram("out", [L, DM], F32, "ExternalOutput")
    hres_d = dram("hres", [L, DM], F32, "Internal")
    wscr_d = dram("wscr", [NG, P, 8, 256], BF16, "Internal")
    woscr_d = dram("woscr", [8, P, 2, DM], BF16, "Internal")
    dbg = {}
    if debug:
        dbg["yssd"] = dram("d_yssd", [P, 8, SP], F32, "ExternalOutput")
        dbg["ys5"] = dram("d_ys5", [P, 4, SP], F32, "ExternalOutput")
        dbg["yret"] = dram("d_yret", [P, 4, SP], F32, "ExternalOutput")

    with ExitStack() as es:
        A = AS(nc, es)
        sb = lambda n, s, d: es.enter_context(nc.sbuf_tensor("s_" + n, s, d))
        pst = lambda n, s, d: es.enter_context(nc.psum_tensor("p_" + n, s, d))

        pT = pst("pT", [P, 1024], BF16)
        pF = [pst("pF0", [P, 512], F32), pst("pF1", [P, 512], F32)]
        pS = pst("pS", [P, 512], F32)
        pY = pst("pY", [P, 1024], F32)
        pC = pst("pC", [P, 1024], F32)
        pTf = pT.bitcast(F32)
        pfi = [0]

        def nextF():
            pfi[0] ^= 1
            return pF[pfi[0]], "pF%d" % pfi[0]

        c32 = sb("c32", [P, NC32], F32)
        cbf = sb("cbf", [P, 512], BF16)
        identb = cbf[:, 0:128]
        onesb = cbf[:, 128:256]
        permb = cbf[:, 256:384]
        blk64b = cbf[:, 384:512]
        U32 = c32[:, C32["U"]:C32["U"] + 128]
        ones32 = c32[:, C32["ones"]:C32["ones"] + 128]
        tri32 = c32[:, C32["tri"]:C32["tri"] + 128]
        dmat = c32[:, C32["dmat"]:C32["dmat"] + 1024].rearrange("p (h t) -> p h t", h=8)
        qdec = c32[:, C32["qdec"]:C32["qdec"] + 256].rearrange("p (a t) -> p a t", a=2)
        kdec = c32[:, C32["kdec"]:C32["kdec"] + 8]
        gch = c32[:, C32["gch"]:C32["gch"] + 2]
        cos_ti = c32[:, C32["cos"]:C32["cos"] + 128]
        sin_ti = c32[:, C32["sin"]:C32["sin"] + 128]
        cc_t = c32[:, C32["cc"]:C32["cc"] + 64]
        sc_t = c32[:, C32["sc"]:C32["sc"] + 64]
        rmask = c32[:, C32["rmask"]:C32["rmask"] + 4]
        Gb = [(pF[0], "pF0"), (pF[1], "pF1"), (pS, "pS"), (pC, "pC")]

        pp = sb("pp", [P, NPP], F32)
        ppc = lambda name, i=0, n=1: pp[:, OFF[name] + i:OFF[name] + i + n]
        cbrow = sb("cbrow", [1, 1280], BF16)
        fnw = sb("fnw", [P, DM], F32)
        wglu = sb("wglu", [P, 4, 512], BF16)
        NSLOT = 4
        wslot = [sb("wslot%d" % i, [P, 8, 256], BF16) for i in range(NSLOT)]
        diagw = sb("diagw", [P, 12, 4, 128], BF16)
        dI = sb("dI", [P, 16, 128], BF16)
        Ab = sb("Ab", [P, 16], F32)
        hbglu = sb("hbglu", [P, 4], F32)
        WG = sb("WG", [P, 4, J, 2, 128], BF16)
        Cl = sb("Cl", [P, 16, J, 2, 32], BF16)
        Ktap = sb("Ktap", [P, 4, J, 128], BF16)
        phc = sb("phc", [P, 16, NB + 1], F32)
        phs = sb("phs", [P, 16, NB + 1], F32)
        Rr = sb("Rr", [P, 16], F32)
        prevT = sb("prevT", [P, 1024], F32)
        prevTb = sb("prevTb", [P, 1024], BF16)
        rstate = sb("rstate", [P, 2, 64], F32)
        rstateb = sb("rstateb", [P, 2, 64], BF16)
        rspad = sb("rspad", [P, 8, 64], BF16)
        Vre = sb("Vre", [P, 16, NB + 1], F32)
        Vim = sb("Vim", [P, 16, NB + 1], F32)
        small = sb("small", [P, 16], F32)
        def TT(e, out, a, b, op, r, w):
            A.op(e, lambda g: g.tensor_tensor(out, a, b, op), r, w)

        def TS(e, out, a, s1, op0, r, w, s2=None, op1=None):
            if op1 is None:
                A.op(e, lambda g: g.tensor_scalar(out, a, s1, None, op0), r, w)
            else:
                A.op(e, lambda g: g.tensor_scalar(out, a, s1, s2, op0, op1), r, w)

        def STT(out, a, s, b, op0, op1, r, w):
            A.op("dve", lambda g: g.scalar_tensor_tensor(out, a, s, b, op0, op1), r, w)

        def ACT(out, a, func, r, w, bias=None, scale=None, accum=None):
            kw = {}
            if bias is not None:
                kw["bias"] = bias
            if scale is not None:
                kw["scale"] = scale
            if accum is not None:
                kw["accum_out"] = accum
            A.op("act", lambda g: g.activation(out, a, func, **kw), r, w)

        def CP(e, out, a, r, w):
            A.op(e, lambda g: g.tensor_copy(out, a), r, w)

        def bc(ap, shape):
            return ap.broadcast_to(shape)

        A.dma("sp", "cst", c32[:], c32_d, writes=["c32"])
        A.dma("pool", "cstb", cbf[:], cbf_d, writes=["cbf"])
        A.dma("sp", "cst", fnw[:], fnw_d.partition_broadcast(P), writes=["fnw"])

        open_stacks = []
        try:
          for l in range(nlayer):
              src_d = x_d if l == 0 else hres_d
              last = (l == nlayer - 1)
              A.barrier()
              es2 = ExitStack()
              open_stacks[:] = [es2]
              sb2 = lambda n, s_, d: es2.enter_context(nc.sbuf_tensor("s_%s_L%d" % (n, l), s_, d))
              pb = sb2("pb", [P, NPB], F32)
              pbc = lambda name: pb[:, OFFB[name]:OFFB[name] + 512]
              A.dma("sp", "pb", pb[:], pb_d[l], writes=["s5p"])
              A.dma("sp", "pp", pp[:], pp_d[l], writes=["pp"])
              A.dma("pool", "cbrow", cbrow[:], cbrow_d[l], writes=["cbrow"])
              A.dma("pool", "wglu", wglu[:], wglu_d[l], writes=["wglu"])
              stg(0.1)
              for gi in range(NG):
                  si = gi % NSLOT
                  A.dma("pool", "wsq%d" % si, wslot[si][:], win_d[l, gi], writes=["ws%d" % si])
                  A.dma("sp", "wscr%d" % gi, wscr_d[gi], wslot[si][:], reads=["ws%d" % si], writes=["wscr%d" % gi])
              for go in range(8):
                  si = (NG + go) % NSLOT
                  wv = wslot[si][:].rearrange("p a b -> p (a b)").rearrange("p (a b) -> p a b", a=2)
                  A.dma("pool", "wsq%d" % si, wv, wout_d[l][:, 2 * go:2 * go + 2, :], writes=["ws%d" % si])
                  A.dma("sp", "woscr%d" % go, woscr_d[go], wv, reads=["ws%d" % si], writes=["woscr%d" % go])
              stg(0.2)
              for tl in range(12):
                  for k in range(4):
                      TS("pool", diagw[:, tl, k, :], identb, ppc("convw", tl * 4 + k), ALU.mult, ["cbf", "pp"], ["diagw"])
              for h in range(16):
                  TS("pool", dI[:, h, :], identb, ppc("dssd", h), ALU.mult, ["cbf", "pp"], ["dI"])
              ACT(Ab[:], ppc("alog", 0, 16), AF.Exp, ["pp"], ["Ab"])
              TS("dve", Ab[:], Ab[:], -1.0, ALU.mult, ["Ab"], ["Ab"])
              TS("dve", hbglu[:], ppc("bglu", 0, 4), 0.5, ALU.mult, ["pp"], ["hbglu"])

              stg(0.3)
              s16 = lambda n: sb2("s5_%s" % n, [P, 16], F32)
              step, lrs, ang, mag, sn, cs, angc, msk = [s16(n) for n in ("step", "lrs", "ang", "mag", "sn", "cs", "angc", "msk")]
              lbr, lbi, den, aa, fre, fim, t16a, t16b = [s16(n) for n in ("lbr", "lbi", "den", "aa", "fre", "fim", "t16a", "t16b")]
              K5 = ["s5p"]

              def e16(e, out, a, b, op):
                  TT(e, out, a, b, op, K5, K5)

              ACT(step[:], ppc("lstep", 0, 16), AF.Exp, ["pp"], K5)
              e16("dve", lrs[:], ppc("lamre", 0, 16), step[:], ALU.mult)
              e16("dve", ang[:], ppc("lamim", 0, 16), step[:], ALU.mult)
              ACT(mag[:], lrs[:], AF.Exp, K5, K5)
              ACT(Rr[:], lrs[:], AF.Exp, K5, K5 + ["Rr"], scale=float(J))
              for _ in range(5):
                  TS("dve", msk[:], ang[:], math.pi, ALU.is_gt, K5, K5, s2=TWO_PI, op1=ALU.mult)
                  e16("dve", ang[:], ang[:], msk[:], ALU.subtract)
              TS("dve", angc[:], ang[:], math.pi / 2, ALU.add, K5, K5)
              TS("dve", msk[:], angc[:], math.pi, ALU.is_gt, K5, K5, s2=TWO_PI, op1=ALU.mult)
              e16("dve", angc[:], angc[:], msk[:], ALU.subtract)
              ACT(sn[:], ang[:], AF.Sin, K5, K5)
              ACT(cs[:], angc[:], AF.Sin, K5, K5)
              e16("dve", lbr[:], mag[:], cs[:], ALU.mult)
              e16("dve", lbi[:], mag[:], sn[:], ALU.mult)
              e16("dve", den[:], ppc("lamre", 0, 16), ppc("lamre", 0, 16), ALU.mult)
              e16("dve", t16a[:], ppc("lamim", 0, 16), ppc("lamim", 0, 16), ALU.mult)
              e16("dve", den[:], den[:], t16a[:], ALU.add)
              A.op("dve", lambda g: g.reciprocal(den[:], den[:]), K5, K5)
              TS("dve", aa[:], lbr[:], -1.0, ALU.add, K5, K5)
              e16("dve", t16a[:], aa[:], ppc("lamre", 0, 16), ALU.mult)
              e16("dve", t16b[:], lbi[:], ppc("lamim", 0, 16), ALU.mult)
              e16("dve", fre[:], t16a[:], t16b[:], ALU.add)
              e16("dve", fre[:], fre[:], den[:], ALU.mult)
              e16("dve", t16a[:], lbi[:], ppc("lamre", 0, 16), ALU.mult)
              e16("dve", t16b[:], aa[:], ppc("lamim", 0, 16), ALU.mult)
              e16("dve", fim[:], t16a[:], t16b[:], ALU.subtract)
              e16("dve", fim[:], fim[:], den[:], ALU.mult)
              s512 = lambda n: sb2("s5_%s" % n, [P, 16, 32], F32)
              bbr, bbi, t5a, t5b, ncim = [s512(n) for n in ("bbr", "bbi", "t5a", "t5b", "ncim")]
              b_re = pbc("bre").rearrange("p (a b) -> p a b", a=16)
              b_im = pbc("bim").rearrange("p (a b) -> p a b", a=16)
              c_re = pbc("cre").rearrange("p (a b) -> p a b", a=16)
              c_im = pbc("cim").rearrange("p (a b) -> p a b", a=16)
              b32 = lambda t: bc(t[:].unsqueeze(2), [P, 16, 32])

              def cmul(o_r, o_i, ar, ai, br, bi, sh_b):
                  TT("dve", t5a[:], ar, sh_b(br), ALU.mult, K5, K5)
                  TT("dve", t5b[:], ai, sh_b(bi), ALU.mult, K5, K5)
                  TT("dve", o_r, t5a[:], t5b[:], ALU.subtract, K5, K5)
                  TT("dve", t5a[:], ar, sh_b(bi), ALU.mult, K5, K5)
                  TT("dve", t5b[:], ai, sh_b(br), ALU.mult, K5, K5)
                  TT("dve", o_i, t5a[:], t5b[:], ALU.add, K5, K5)

              cmul(bbr[:], bbi[:], b_re, b_im, fre, fim, b32)
              TS("dve", ncim[:], c_im, -1.0, ALU.mult, K5, K5)
              Xr = [bbr] + [s512("xr%d" % k) for k in range(1, J)]
              Xi = [bbi] + [s512("xi%d" % k) for k in range(1, J)]
              for k in range(1, J):
                  cmul(Xr[k][:], Xi[k][:], Xr[k - 1][:], Xi[k - 1][:], lbr, lbi, b32)
              clr_prev, cli_prev = c_re, c_im
              clr = [s512("clr%d" % k) for k in range(J)]
              cli = [s512("cli%d" % k) for k in range(J)]
              for ti in range(J):
                  cmul(clr[ti][:], cli[ti][:], clr_prev, cli_prev, lbr, lbi, b32)
                  clr_prev, cli_prev = clr[ti][:], cli[ti][:]
                  CP("dve", Cl[:, :, ti, 0, :], clr[ti][:], K5, ["Cl"])
                  TS("dve", Cl[:, :, ti, 1, :], cli[ti][:], -1.0, ALU.mult, K5, ["Cl"])
              Xrb = [sb2("s5_xrb%d" % k, [P, 16, 32], BF16) for k in range(J)]
              Xib = [sb2("s5_xib%d" % k, [P, 16, 32], BF16) for k in range(J)]
              for k in range(J):
                  CP("dve", Xrb[k][:], Xr[k][:], K5, K5)
                  CP("dve", Xib[k][:], Xi[k][:], K5, K5)
              stg(0.4)
              for pair in range(16):
                  q, r = pair // 4, pair % 4
                  for ti in range(J):
                      for part, Xb in ((0, Xrb), (1, Xib)):
                          pf, pk = nextF()
                          A.op("pe", lambda g: g.matmul(pf[32 * r:32 * r + 32, 0:128], Xb[J - 1 - ti][:, pair, :], identb,
                                                         start=True, stop=True, tile_position=(0, 32 * r)),
                               K5 + ["cbf"], [pk])
                          CP("dve", WG[32 * r:32 * r + 32, q, ti, part, :], pf[32 * r:32 * r + 32, 0:128], [pk], ["WG"])
              A.op("pool", lambda g: g.memset(Ktap[:], 0.0), [], ["Ktap"])
              for q in range(4):
                  for j in range(J):
                      pf, pk = nextF()
                      A.group("pe", [
                          lambda g: g.matmul(pf[:, 0:128], Xr[j][:, 4 * q:4 * q + 4, :].rearrange("p a b -> p (a b)"),
                                             c_re[:, 4 * q:4 * q + 4, :].rearrange("p a b -> p (a b)"), start=True, stop=False),
                          lambda g: g.matmul(pf[:, 0:128], Xi[j][:, 4 * q:4 * q + 4, :].rearrange("p a b -> p (a b)"),
                                             ncim[:, 4 * q:4 * q + 4, :].rearrange("p a b -> p (a b)"), start=False, stop=True),
                      ], K5, [pk])
                      for r in range(4):
                          CP("dve", Ktap[32 * r:32 * r + 32, q, j, 32 * r:32 * r + 32], pf[32 * r:32 * r + 32, 32 * r:32 * r + 32], [pk], ["Ktap"])
              eJr, eJi = s16("eJr"), s16("eJi")
              CP("dve", eJr[:], cs[:], K5, K5)
              CP("dve", eJi[:], sn[:], K5, K5)
              for _ in range(J - 1):
                  e16("dve", t16a[:], eJr[:], cs[:], ALU.mult)
                  e16("dve", t16b[:], eJi[:], sn[:], ALU.mult)
                  e16("dve", aa[:], t16a[:], t16b[:], ALU.subtract)
                  e16("dve", t16a[:], eJr[:], sn[:], ALU.mult)
                  e16("dve", t16b[:], eJi[:], cs[:], ALU.mult)
                  e16("dve", eJi[:], t16a[:], t16b[:], ALU.add)
                  CP("dve", eJr[:], aa[:], K5, K5)
              KP = ["ph"]
              A.op("dve", lambda g: g.memset(phc[:, :, 0:1], 1.0), [], KP)
              A.op("dve", lambda g: g.memset(phs[:, :, 0:1], 0.0), [], KP)
              CP("dve", phc[:, :, 1], eJr[:], K5, KP)
              CP("dve", phs[:, :, 1], eJi[:], K5, KP)
              tph = sb2("s5_tph", [P, 16, 32], F32)
              tph2 = sb2("s5_tph2", [P, 16, 32], F32)
              n = 1
              while n < NB:
                  cn = bc(phc[:, :, n:n + 1], [P, 16, n])
                  sn_ = bc(phs[:, :, n:n + 1], [P, 16, n])
                  TT("dve", tph[:, :, 0:n], phc[:, :, 1:n + 1], cn, ALU.mult, KP, KP)
                  TT("dve", tph2[:, :, 0:n], phs[:, :, 1:n + 1], sn_, ALU.mult, KP, KP)
                  TT("dve", phc[:, :, n + 1:2 * n + 1], tph[:, :, 0:n], tph2[:, :, 0:n], ALU.subtract, KP, KP)
                  TT("dve", tph[:, :, 0:n], phc[:, :, 1:n + 1], sn_, ALU.mult, KP, KP)
                  TT("dve", tph2[:, :, 0:n], phs[:, :, 1:n + 1], cn, ALU.mult, KP, KP)
                  TT("dve", phs[:, :, n + 1:2 * n + 1], tph[:, :, 0:n], tph2[:, :, 0:n], ALU.add, KP, KP)
                  n *= 2

              stg(1)
              A.barrier()
              es2.close()
              es3 = ExitStack()
              open_stacks[:] = [es3]
              sb3 = lambda n, s_, d: es3.enter_context(nc.sbuf_tensor("s_%s_L%d" % (n, l), s_, d))
              xt = [sb3("xt0", [P, DM], F32)] * 2
              xn = sb3("xn", [P, DM], BF16)
              hTs = [sb3("hT0", [P, 8, SP], BF16), sb3("hT1", [P, 8, SP], BF16)]
              zs = sb3("zs", [P, 8, SP], BF16)
              xbcT = sb3("xbcT", [P, 12, 3 + SP], BF16)
              g5 = sb3("g5", [P, 4, SP], BF16)
              uT = sb3("uT", [P, 4, SP], BF16)
              qT = sb3("qT", [P, 2, SP], BF16)
              kT = sb3("kT", [P, 2, SP], BF16)
              rg = sb3("rg", [P, 4, SP], BF16)
              vtok = [sb3("vtok%d" % i, [P, 512], BF16) for i in range(NCH)]
              dtt = [sb3("dt%d" % i, [P, 16], F32) for i in range(NCH)]
              dtA = [sb3("dtA%d" % i, [P, 16], F32) for i in range(NCH)]
              BCT = sb3("BCT", [P, 4, SP], BF16)
              yTs5 = sb3("yTs5", [P, 4, SP], BF16)
              cre = sb3("cre", [P, 16, NB], F32)
              cim = sb3("cim", [P, 16, NB], F32)
              Sre = sb3("Sre", [P, 16, NB], BF16)
              Sim = sb3("Sim", [P, 16, NB], BF16)
              tA = sb3("tA", [P, 1024], F32)
              tB = sb3("tB", [P, 1024], F32)
              y5a = sb3("y5a", [P, 4, SP], F32)
              y5b = sb3("y5b", [P, 4, SP], BF16)
              xstok = sb3("xstok", [P, 1024], BF16)
              xsw = sb3("xsw", [P, 1024], BF16)
              Btok = sb3("Btok", [P, 256], BF16)
              cbTm = sb3("cbTm", [P, 2, 128], F32)
              Dq_2 = [sb3("Dq0", [P, 4, 128], F32)] * 2
              dec_2 = [sb3("dec0", [P, 4, 128], F32), sb3("dec1", [P, 4, 128], F32)]
              eac_2 = [sb3("eac0", [P, 4, 128], F32), sb3("eac1", [P, 4, 128], F32)]
              Mq_2 = [sb3("Mq0", [P, 4, 128], BF16), sb3("Mq1", [P, 4, 128], BF16)]
              CTs_2 = [sb3("CTs0", [P, 4, 128], BF16), sb3("CTs1", [P, 4, 128], BF16)]
              y1 = sb3("y1", [P, 8, 128], F32)
              ysq = sb3("ysq", [P, 8, 128], BF16)
              rstd2 = sb3("rstd2", [P, 2, 128], F32)
              yTssd = sb3("yTssd", [P, 8, SP], BF16)
              wls = sb3("wls", [P, 16], F32)
              cosT = sb3("cosT", [P, 128], F32)
              sinT = sb3("sinT", [P, 128], F32)
              qb = sb3("qb", [P, 2, 128], BF16)
              qdb = sb3("qdb", [P, 2, 128], BF16)
              kb = sb3("kb", [P, 2, 128], BF16)
              kdtok = sb3("kdtok", [P, 256], BF16)
              ST = sb3("ST", [P, 8, 128], BF16)
              yr1 = sb3("yr1", [P, 4, 128], F32)
              yrsq = sb3("yrsq", [P, 4, 128], BF16)
              rstd4 = sb3("rstd4", [P, 4, 128], F32)
              yTret = sb3("yTret", [P, 4, SP], BF16)
              hn = sb3("hn", [P, DM], F32)
              dbt = tB[:].rearrange("p (a t) -> p a t", a=8)
              A.op("pool", lambda g: g.memset(prevT[:], 0.0), [], ["prevT"])
              A.op("pool", lambda g: g.memset(prevTb[:], 0.0), [], ["prevTb"])
              A.op("pool", lambda g: g.memset(rstate[:], 0.0), [], ["rstate"])
              A.op("pool", lambda g: g.memset(rstateb[:], 0.0), [], ["rstateb"])
              A.op("pool", lambda g: g.memset(rspad[:], 0.0), [], ["rspad"])
              A.op("pool", lambda g: g.memset(Vre[:], 0.0), [], ["Vre"])
              A.op("pool", lambda g: g.memset(Vim[:], 0.0), [], ["Vim"])
              A.op("pool", lambda g: g.memset(xbcT[:, :, 0:3], 0.0), [], ["xbcT"])

              def front_end(s):
                  t0 = s * SP
                  hT = hTs[s % 2]
                  hk = "hT%d" % (s % 2)
                  for c in range(NCH):
                      xk = "xt0"
                      A.dma("sp", xk, xt[c][:], src_d[t0 + c * T:t0 + (c + 1) * T, :], writes=[xk])
                      ACT(xn[:], xt[c][:], AF.Square, [xk], ["xn", "small"], accum=small[:, 0:1])
                      ACT(small[:, 1:2], small[:, 0:1], AF.Ln, ["small"], ["small"], bias=EPS, scale=1.0 / DM)
                      ACT(small[:, 2:3], small[:, 1:2], AF.Exp, ["small"], ["small"], scale=-0.5)
                      ACT(xn[:], xt[c][:], AF.Copy, [xk, "small"], ["xn"], scale=small[:, 2:3])
                      A.group("pe", [(lambda g, kt=kt: g.transpose(pT[:, kt * 128:(kt + 1) * 128], xn[:, kt * 128:(kt + 1) * 128], identb))
                                     for kt in range(8)], ["xn", "cbf"], ["pT"])
                      TT("dve", hT[:, :, c * T:(c + 1) * T], pT[:].rearrange("p (k t) -> p k t", k=8),
                         bc(ppc("normw", 0, 8).unsqueeze(2), [P, 8, T]), ALU.mult, ["pT", "pp"], [hk])

              front_end(0)
              pend = [None]
              for s in range(nspan):
                  t0 = s * SP
                  hT = hTs[s % 2]
                  hk = "hT%d" % (s % 2)
                  for gi in range(NG):
                      if gi == 3 and pend[0] is not None:
                          pend[0]()
                          pend[0] = None
                      si = gi % NSLOT
                      wk = "ws%d" % si
                      A.dma("sp", wk, wslot[si][:], wscr_d[gi], reads=["wscr%d" % gi], writes=[wk])
                      if gi < 18:
                          for tt in range(2):
                              ft = 2 * gi + tt
                              pf, pk = nextF()
                              A.group("pe", [(lambda g, kt=kt: g.matmul(pf[:, 0:SP], wslot[si][:, kt, tt * 128:(tt + 1) * 128], hT[:, kt, :],
                                                                        start=(kt == 0), stop=(kt == 7))) for kt in range(8)],
                                      [wk, hk], [pk])
                              if ft < 8:
                                  ACT(zs[:, ft, :], pf[:, 0:SP], AF.Silu, [pk], ["zs"])
                              elif ft < 20:
                                  CP("dve", xbcT[:, ft - 8, 3:3 + SP], pf[:, 0:SP], [pk], ["xbcT"])
                              elif ft < 24:
                                  ACT(g5[:, ft - 20, :], pf[:, 0:SP], AF.Silu, [pk], ["g5"])
                              elif ft < 28:
                                  CP("dve", uT[:, ft - 24, :], pf[:, 0:SP], [pk], ["uT"])
                              elif ft < 30:
                                  CP("dve", qT[:, ft - 28, :], pf[:, 0:SP], [pk], ["qT"])
                              elif ft < 32:
                                  CP("dve", kT[:, ft - 30, :], pf[:, 0:SP], [pk], ["kT"])
                              else:
                                  ACT(rg[:, ft - 32, :], pf[:, 0:SP], AF.Silu, [pk], ["rg"])
                      elif gi < 20:
                          hv = gi - 18
                          for c in range(NCH):
                              pf, pk = nextF()
                              A.group("pe", [(lambda g, kt=kt: g.matmul(pf[:, 0:256], hT[:, kt, c * T:(c + 1) * T], wslot[si][:, kt, :],
                                                                        start=(kt == 0), stop=(kt == 7))) for kt in range(8)],
                                      [wk, hk], [pk])
                              CP("dve", vtok[c][:, hv * 256:(hv + 1) * 256], pf[:, 0:256], [pk], ["vtok%d" % c])
                      else:
                          for c in range(NCH):
                              pf, pk = nextF()
                              A.group("pe", [(lambda g, kt=kt: g.matmul(pf[:, 0:16], hT[:, kt, c * T:(c + 1) * T], wslot[si][:, kt, 0:16],
                                                                        start=(kt == 0), stop=(kt == 7))) for kt in range(8)],
                                      [wk, hk], [pk])
                              dk = "dt%d" % c
                              TT("dve", dtt[c][:], pf[:, 0:16], ppc("dtb", 0, 16), ALU.add, [pk, "pp"], [dk])
                              ACT(dtt[c][:], dtt[c][:], AF.Exp, [dk], [dk])
                              ACT(dtt[c][:], dtt[c][:], AF.Ln, [dk], [dk], bias=1.0)
                              TT("dve", dtA[c][:], dtt[c][:], Ab[:], ALU.mult, [dk, "Ab"], ["dtA%d" % c])

                  if s + 1 < nspan:
                      front_end(s + 1)
                  stg(2)
                  for i4 in range(4):
                      tl = 8 + i4
                      pf, pk = nextF()
                      A.group("pe", [(lambda g, k=k: g.matmul(pf[:, 0:SP], diagw[:, tl, k, :], xbcT[:, tl, k:k + SP],
                                                              start=(k == 0), stop=(k == 3))) for k in range(4)],
                              ["diagw", "xbcT"], [pk])
                      ACT(BCT[:, i4, :], pf[:, 0:SP], AF.Silu, [pk, "pp"], ["BCT"], bias=ppc("convb", tl))

                  stg(2.5)
                  fns = []
                  for bt in range(4):
                      for part in range(2):
                          for ti in range(J):
                              for r in range(4):
                                  uv = uT[32 * r:32 * r + 32, bt, :].rearrange("p (b j) -> p b j", j=J)
                                  fns.append(lambda g, r=r, part=part, ti=ti, uv=uv, bt=bt: g.matmul(
                                      Gb[r][0][:, (bt * 2 + part) * NB:(bt * 2 + part + 1) * NB], WG[32 * r:32 * r + 32, bt, ti, part, :],
                                      uv[:, :, ti], start=(ti == 0), stop=(ti == J - 1), tile_position=(32 * r, 0)))
                  A.group("pe", fns, ["WG", "uT"], ["pF0", "pF1", "pS", "pC"])
                  for r in range(4):
                      gk = Gb[r][1]
                      Gv = Gb[r][0][:, 0:512].rearrange("p (t a b) -> p t a b", t=4, a=2)
                      tAv = tA[:, 0:4 * NB].rearrange("p (r b) -> p r b", r=4)
                      tBv = tB[:, 0:4 * NB].rearrange("p (r b) -> p r b", r=4)
                      TT("dve", tAv, Gv[:, :, 0, :], phc[:, r:16:4, 1:NB + 1], ALU.mult, [gk, "ph"], ["tA"])
                      TT("dve", tBv, Gv[:, :, 1, :], phs[:, r:16:4, 1:NB + 1], ALU.mult, [gk, "ph"], ["tB"])
                      TT("dve", cre[:, r:16:4, :], tAv, tBv, ALU.add, ["tA", "tB"], ["cre"])
                      TT("dve", tAv, Gv[:, :, 1, :], phc[:, r:16:4, 1:NB + 1], ALU.mult, [gk, "ph"], ["tA"])
                      TT("dve", tBv, Gv[:, :, 0, :], phs[:, r:16:4, 1:NB + 1], ALU.mult, [gk, "ph"], ["tB"])
                      TT("dve", cim[:, r:16:4, :], tAv, tBv, ALU.subtract, ["tA", "tB"], ["cim"])
                  stg(2.6)
                  for pair in range(16):
                      A.op("dve", lambda g: g.tensor_tensor_scan(Vre[:, pair, 1:NB + 1], bc(Rr[:, pair:pair + 1], [P, NB]), cre[:, pair, :],
                                                                 Vre[:, pair, 0:1], ALU.mult, ALU.add), ["Rr", "cre", "Vre"], ["Vre"])
                      A.op("dve", lambda g: g.tensor_tensor_scan(Vim[:, pair, 1:NB + 1], bc(Rr[:, pair:pair + 1], [P, NB]), cim[:, pair, :],
                                                                 Vim[:, pair, 0:1], ALU.mult, ALU.add), ["Rr", "cim", "Vim"], ["Vim"])
                  tA3 = tA[:].rearrange("p (a b) -> p a b", a=16)
                  tB3 = tB[:].rearrange("p (a b) -> p a b", a=16)
                  TT("dve", tA3, Vre[:, :, 0:NB], phc[:, :, 0:NB], ALU.mult, ["Vre", "ph"], ["tA"])
                  TT("dve", tB3, Vim[:, :, 0:NB], phs[:, :, 0:NB], ALU.mult, ["Vim", "ph"], ["tB"])
                  TT("dve", Sre[:], tA3, tB3, ALU.subtract, ["tA", "tB"], ["Sre"])
                  TT("dve", tA3, Vre[:, :, 0:NB], phs[:, :, 0:NB], ALU.mult, ["Vre", "ph"], ["tA"])
                  TT("dve", tB3, Vim[:, :, 0:NB], phc[:, :, 0:NB], ALU.mult, ["Vim", "ph"], ["tB"])
                  TT("dve", Sim[:], tA3, tB3, ALU.add, ["tA", "tB"], ["Sim"])
                  tcr = tA[:, 0:16]
                  tci = tB[:, 0:16]
                  tc2 = tA[:, 16:32]
                  tc3 = tB[:, 16:32]
                  TT("dve", tcr, Vre[:, :, NB], phc[:, :, NB], ALU.mult, ["Vre", "ph", "Sre", "Sim"], ["tA"])
                  TT("dve", tci, Vim[:, :, NB], phs[:, :, NB], ALU.mult, ["Vim", "ph", "Sre", "Sim"], ["tB"])
                  TT("dve", tc2, Vre[:, :, NB], phs[:, :, NB], ALU.mult, ["Vre", "ph"], ["tA"])
                  TT("dve", tc3, Vim[:, :, NB], phc[:, :, NB], ALU.mult, ["Vim", "ph"], ["tB"])
                  TT("dve", Vre[:, :, 0], tcr, tci, ALU.subtract, ["tA", "tB", "Sre", "Sim"], ["Vre"])
                  TT("dve", Vim[:, :, 0], tc2, tc3, ALU.add, ["tA", "tB", "Sre", "Sim"], ["Vim"])
                  stg(2.7)
                  for q in range(4):
                      pf, pk = nextF()
                      ov = pf[:, 0:SP].rearrange("p (b j) -> p b j", j=J)
                      uvq = uT[:, q, :].rearrange("p (b j) -> p b j", j=J)
                      fns = []
                      fns.append(lambda g: g.matmul(pf[:, 0:SP], Ktap[:, q, 0, :], uT[:, q, :], start=True, stop=True))
                      for j in range(1, J):
                          for ti in range(j, J):
                              fns.append(lambda g, j=j, ti=ti: g.matmul(ov[:, :, ti], Ktap[:, q, j, :], uvq[:, :, ti - j],
                                                                        start=False, stop=True, skip_group_check=True))
                      for r in range(4):
                          pair = 4 * q + r
                          ovr = pf[32 * r:32 * r + 32, 0:SP].rearrange("p (b j) -> p b j", j=J)
                          for ti in range(J):
                              for part, Sx in ((0, Sre), (1, Sim)):
                                  lastmm = (r == 3 and ti == J - 1 and part == 1)
                                  fns.append(lambda g, pair=pair, ti=ti, part=part, Sx=Sx, ovr=ovr, lastmm=lastmm, r=r: g.matmul(
                                      ovr[:, :, ti], Cl[:, pair, ti, part, :], Sx[:, pair, :], start=False, stop=True, skip_group_check=True,
                                      tile_position=(0, 32 * r)))
                      A.group("pe", fns, ["Ktap", "uT", "Cl", "Sre", "Sim"], [pk])
                      STT(y5a[:, q, :], uT[:, q, :], ppc("s5d", q), pf[:, 0:SP], ALU.mult, ALU.add, ["uT", "pp", pk], ["y5a"])
                  stg(2.8)
                  ACT(y5a[:], y5a[:], AF.Gelu_apprx_tanh, ["y5a"], ["y5a"])
                  A.op("act", lambda g: g.copy(y5b[:], y5a[:]), ["y5a"], ["y5b"])
                  for jt in range(4):
                      pf, pk = nextF()
                      A.group("pe", [(lambda g, kt=kt: g.matmul(pf[:, 0:SP], wglu[:, kt, jt * 128:(jt + 1) * 128], y5b[:, kt, :],
                                                                start=(kt == 0), stop=(kt == 3))) for kt in range(4)],
                              ["wglu", "y5b"], [pk])
                      ACT(tA[:, 0:SP], pf[:, 0:SP], AF.Tanh, [pk, "hbglu"], ["tA"], bias=hbglu[:, jt:jt + 1], scale=0.5)
                      TS("dve", tA[:, 0:SP], tA[:, 0:SP], 0.5, ALU.mult, ["tA"], ["tA"], s2=0.5, op1=ALU.add)
                      TT("dve", tA[:, 0:SP], tA[:, 0:SP], y5a[:, jt, :], ALU.mult, ["tA", "y5a"], ["tA"])
                      TT("dve", yTs5[:, jt, :], tA[:, 0:SP], g5[:, jt, :], ALU.mult, ["tA", "g5"], ["yTs5"])
                      if debug and s == 0 and l == 0:
                          TT("dve", tB[:, 0:SP], tA[:, 0:SP], g5[:, jt, :], ALU.mult, ["tA", "g5"], ["tB"])
                          A.dma("sp", "dbg", dbg["ys5"][:, jt, :], tB[:, 0:SP], reads=["tB"], writes=["dbg_out"])

                  stg(3)
                  for c in range(NCH):
                      cs_ = slice(c * T, (c + 1) * T)
                      gc = s * NCH + c
                      dk, dak, vk = "dt%d" % c, "dtA%d" % c, "vtok%d" % c
                      fns = []
                      for tl in range(8):
                          for k in range(4):
                              fns.append(lambda g, tl=tl, k=k: g.matmul(pC[:, tl * 128:(tl + 1) * 128], xbcT[:, tl, c * T + k:c * T + k + T],
                                                                        diagw[:, tl, k, :], start=(k == 0), stop=False))
                          fns.append(lambda g, tl=tl: g.matmul(pC[:, tl * 128:(tl + 1) * 128], onesb[0:1, :], cbrow[0:1, tl * 128:(tl + 1) * 128],
                                                               start=False, stop=True))
                      A.group("pe", fns, ["xbcT", "diagw", "cbf", "cbrow"], ["pC"])
                      ACT(xstok[:], pC[:], AF.Silu, ["pC"], ["xstok"])
                      pf, pk = nextF()
                      fns = []
                      for i2 in range(2):
                          tl = 8 + i2
                          for k in range(4):
                              fns.append(lambda g, tl=tl, k=k, i2=i2: g.matmul(pf[:, i2 * 128:(i2 + 1) * 128], xbcT[:, tl, c * T + k:c * T + k + T],
                                                                               diagw[:, tl, k, :], start=(k == 0), stop=False))
                          fns.append(lambda g, tl=tl, i2=i2: g.matmul(pf[:, i2 * 128:(i2 + 1) * 128], onesb[0:1, :], cbrow[0:1, tl * 128:(tl + 1) * 128],
                                                                      start=False, stop=True))
                      A.group("pe", fns, ["xbcT", "diagw", "cbf", "cbrow"], [pk])
                      ACT(Btok[:], pf[:, 0:256], AF.Silu, [pk], ["Btok"])
                      pf, pk = nextF()
                      A.op("pe", lambda g: g.matmul(pf[:, 0:16], U32, dtA[c][:], start=True, stop=True), ["c32", dak], [pk])
                      ACT(wls[:], pf[:, 0:16], AF.Exp, [pk], ["wls"])
                      TT("dve", wls[:], wls[:], dtt[c][:], ALU.mult, ["wls", dk], ["wls"])
                      TT("dve", xsw[:].rearrange("p (h d) -> p h d", h=16), xstok[:].rearrange("p (h d) -> p h d", h=16),
                         bc(wls[:].unsqueeze(2), [P, 16, 64]), ALU.mult, ["xstok", "wls"], ["xsw"])
                      pf, pk = nextF()
                      A.group("pe", [(lambda g, gg=gg: g.matmul(pf[:, gg * 128:(gg + 1) * 128], BCT[:, gg, cs_], BCT[:, 2 + gg, cs_],
                                                                start=True, stop=True)) for gg in range(2)], ["BCT"], [pk])
                      TT("dve", cbTm[:], pf[:, 0:256].rearrange("p (g t) -> p g t", g=2), bc(tri32.unsqueeze(1), [P, 2, 128]), ALU.mult,
                         [pk, "c32"], ["cbTm"])
                      for qd in range(4):
                          gg = qd // 2
                          pq = qd % 2
                          Dq, dec, eac, Mq, CTs = Dq_2[pq], dec_2[pq], eac_2[pq], Mq_2[pq], CTs_2[pq]
                          kDq, kdecq, keac, kMq, kCTs = "Dq0", "dec%d" % pq, "eac%d" % pq, "Mq%d" % pq, "CTs%d" % pq
                          TT("dve", Dq[:], bc(tri32.unsqueeze(1), [P, 4, 128]), bc(dtA[c][:, 4 * qd:4 * qd + 4].unsqueeze(2), [P, 4, 128]),
                             ALU.mult, ["c32", dak], [kDq])
                          Dq2 = Dq[:].rearrange("p h t -> p (h t)")
                          A.op("pe", lambda g: g.matmul(pS[:], U32, Dq2, start=True, stop=True), ["c32", kDq], ["pS"])
                          ACT(dec[:].rearrange("p h t -> p (h t)"), pS[:], AF.Exp, ["pS"], [kdecq])
                          A.op("pe", lambda g: g.matmul(pTf[:], ones32, Dq2, start=True, stop=True), ["c32", kDq], ["pT"])
                          ACT(eac[:].rearrange("p h t -> p (h t)"), pTf[:], AF.Exp, ["pT"], [keac])
                          for hh in range(4):
                              h = 4 * qd + hh
                              STT(Mq[:, hh, :], dec[:, hh, :], dtt[c][:, h:h + 1], cbTm[:, gg, :], ALU.mult, ALU.mult,
                                  [kdecq, dk, "cbTm"], [kMq])
                          TT("dve", CTs[:], bc(BCT[:, 2 + gg, cs_].unsqueeze(1), [P, 4, 128]), eac[:], ALU.mult, ["BCT", keac], [kCTs])
                          fns = []
                          for hh in range(4):
                              h = 4 * qd + hh
                              po = pY[64 * (h % 2):64 * (h % 2) + 64, (h // 2) * 128:(h // 2 + 1) * 128]
                              tp = (0, 64 * (h % 2))
                              fns.append(lambda g, h=h, hh=hh, po=po, tp=tp: g.matmul(po, xstok[:, h * 64:(h + 1) * 64], Mq[:, hh, :],
                                                                                      start=True, stop=False, tile_position=tp))
                              fns.append(lambda g, h=h, po=po, tp=tp: g.matmul(po, xstok[:, h * 64:(h + 1) * 64], dI[:, h, :],
                                                                               start=False, stop=False, tile_position=tp))
                              fns.append(lambda g, h=h, hh=hh, po=po, tp=tp: g.matmul(po, prevTb[:, h * 64:(h + 1) * 64], CTs[:, hh, :],
                                                                                      start=False, stop=True, tile_position=tp))
                          A.group("pe", fns, ["xstok", kMq, "dI", "prevTb", kCTs], ["pY"])
                          pvq = prevT[:, qd * 256:(qd + 1) * 256].rearrange("p (h d) -> p h d", h=4)
                          TT("dve", pvq, pvq, bc(eac[:, :, 127:128], [P, 4, 64]), ALU.mult, ["prevT", keac], ["prevT"])
                      A.group("pe", [(lambda g, gg=gg: g.matmul(pC[:, gg * 512:(gg + 1) * 512], Btok[:, gg * 128:(gg + 1) * 128],
                                                                xsw[:, gg * 512:(gg + 1) * 512], start=True, stop=True)) for gg in range(2)],
                              ["Btok", "xsw"], ["pC"])
                      TT("dve", prevT[:], prevT[:], pC[:], ALU.add, ["prevT", "pC"], ["prevT"])
                      A.op("act", lambda g: g.copy(prevTb[:], prevT[:]), ["prevT"], ["prevTb"])
                      TT("dve", y1[:], pY[:].rearrange("p (a t) -> p a t", a=8), zs[:, :, cs_], ALU.mult, ["pY", "zs"], ["y1"])
                      ACT(ysq[:], y1[:], AF.Square, ["y1"], ["ysq"])
                      pf, pk = nextF()
                      fns = []
                      for gg in range(2):
                          for i in range(4):
                              fns.append(lambda g, gg=gg, i=i: g.matmul(pf[:, gg * 128:(gg + 1) * 128], onesb, ysq[:, 4 * gg + i, :],
                                                                        start=(i == 0), stop=(i == 3)))
                      A.group("pe", fns, ["cbf", "ysq"], [pk])
                      r2 = rstd2[:].rearrange("p g t -> p (g t)")
                      ACT(r2, pf[:, 0:256], AF.Ln, [pk], ["rstd2"], bias=EPS, scale=1.0 / 512)
                      ACT(r2, r2, AF.Exp, ["rstd2"], ["rstd2"], scale=-0.5)
                      TT("dve", y1[:], y1[:], bc(ppc("ssdnw", 0, 8).unsqueeze(2), [P, 8, 128]), ALU.mult, ["y1", "pp"], ["y1"])
                      for gg in range(2):
                          TT("dve", yTssd[:, 4 * gg:4 * gg + 4, cs_], y1[:, 4 * gg:4 * gg + 4, :], bc(rstd2[:, gg:gg + 1, :], [P, 4, 128]), ALU.mult,
                             ["y1", "rstd2"], ["yTssd"])
                      if debug and s == 0 and l == 0:
                          CP("dve", dbt, yTssd[:, :, cs_], ["yTssd"], ["tB"])
                          A.dma("sp", "dbg", dbg["yssd"][:, :, cs_], dbt, reads=["tB"], writes=["dbg_out"])

                      stg(4)
                      TS("dve", cosT[:], sin_ti, sc_t[:, gc:gc + 1], ALU.mult, ["c32"], ["cosT"])
                      STT(cosT[:], cos_ti, cc_t[:, gc:gc + 1], cosT[:], ALU.mult, ALU.subtract, ["c32", "cosT"], ["cosT"])
                      TS("dve", sinT[:], cos_ti, sc_t[:, gc:gc + 1], ALU.mult, ["c32"], ["sinT"])
                      STT(sinT[:], sin_ti, cc_t[:, gc:gc + 1], sinT[:], ALU.mult, ALU.add, ["c32", "sinT"], ["sinT"])
                      for which, XT_, xkey in (("q", qT, "qT"), ("k", kT, "kT")):
                          for tl in range(2):
                              pf, pk = nextF()
                              A.op("pe", lambda g: g.matmul(pf[:, 0:128], permb, XT_[:, tl, cs_], start=True, stop=True), ["cbf", xkey], [pk])
                              TT("dve", tA[:, 0:128], XT_[:, tl, cs_], cosT[:], ALU.mult, [xkey, "cosT"], ["tA"])
                              TT("dve", tB[:, 0:128], pf[:, 0:128], sinT[:], ALU.mult, [pk, "sinT"], ["tB"])
                              if which == "q":
                                  TT("dve", tA[:, 0:128], tA[:, 0:128], tB[:, 0:128], ALU.add, ["tA", "tB"], ["tA"])
                                  A.op("act", lambda g: g.copy(qb[:, tl, :], tA[:, 0:128]), ["tA"], ["qb"])
                                  TT("dve", qdb[:, tl, :], tA[:, 0:128], qdec[:, tl, :], ALU.mult, ["tA", "c32"], ["qdb"])
                              else:
                                  TT("dve", kb[:, tl, :], tA[:, 0:128], tB[:, 0:128], ALU.add, ["tA", "tB"], ["kb"])
                      A.group("pe", [(lambda g, tl=tl: g.transpose(pT[:, tl * 128:(tl + 1) * 128], kb[:, tl, :], identb)) for tl in range(2)],
                              ["kb", "cbf"], ["pT"])
                      TT("dve", kdtok[:].rearrange("p (h d) -> p h d", h=8), pT[:, 0:256].rearrange("p (h d) -> p h d", h=8),
                         bc(kdec.unsqueeze(2), [P, 8, 32]), ALU.mult, ["pT", "c32"], ["kdtok"])
                      fns = []
                      for h in range(8):
                          r, tl = h % 4, h // 4
                          fns.append(lambda g, h=h, r=r, tl=tl: g.matmul(Gb[r][0][:, tl * 128:(tl + 1) * 128], kb[32 * r:32 * r + 32, tl, :],
                                                                         qb[32 * r:32 * r + 32, tl, :], start=True, stop=True,
                                                                         tile_position=(32 * r, 0)))
                      A.group("pe", fns, ["kb", "qb"], ["pF0", "pF1", "pS", "pC"])
                      for r in range(4):
                          TT("dve", ST[:, r:8:4, :], Gb[r][0][:, 0:256].rearrange("p (a t) -> p a t", a=2), dmat[:, r:8:4, :], ALU.mult,
                             [Gb[r][1], "c32"], ["ST"])
                      pfy, pky = nextF()
                      fns = []
                      for h in range(8):
                          r, tl = h % 4, h // 4
                          po = pfy[64 * (h % 2):64 * (h % 2) + 64, (h // 2) * 128:(h // 2 + 1) * 128]
                          fns.append(lambda g, h=h, po=po: g.matmul(po, vtok[c][:, h * 64:(h + 1) * 64], ST[:, h, :], start=True, stop=False,
                                                                    tile_position=(0, 64 * (h % 2))))
                          fns.append(lambda g, h=h, po=po, r=r, tl=tl: g.matmul(po, rspad[:, h, :], qdb[:, tl, :],
                                                                                start=False, stop=True, tile_position=(0, 64 * (h % 2))))
                      A.group("pe", fns, [vk, "ST", "rspad", "qdb"], [pky])
                      pf, pk = nextF()
                      fns = []
                      for h in range(8):
                          r, tl = h % 4, h // 4
                          fns.append(lambda g, h=h, r=r, tl=tl: g.matmul(pf[32 * r:32 * r + 32, tl * 64:(tl + 1) * 64], kdtok[:, h * 32:(h + 1) * 32],
                                                                         vtok[c][:, h * 64:(h + 1) * 64], start=True, stop=True,
                                                                         tile_position=(0, 32 * r)))
                      A.group("pe", fns, ["kdtok", vk], [pk])
                      for tl in range(2):
                          STT(rstate[:, tl, :], rstate[:, tl, :], gch[:, tl:tl + 1], pf[:, tl * 64:(tl + 1) * 64], ALU.mult, ALU.add,
                              ["rstate", "c32", pk], ["rstate"])
                      for h in range(8):
                          TS("pool", rspad[:, h, :], rstate[:, h // 4, :], rmask[:, h % 4:h % 4 + 1], ALU.mult, ["rstate", "c32"], ["rspad"])
                      CP("dve", yr1[:].rearrange("p a t -> p (a t)"), pfy[:], [pky], ["yr1"])
                      ACT(yrsq[:], yr1[:], AF.Square, ["yr1"], ["yrsq"])
                      pf, pk = nextF()
                      A.group("pe", [(lambda g, i=i: g.matmul(pf[:, i * 128:(i + 1) * 128], blk64b, yrsq[:, i, :], start=True, stop=True))
                                     for i in range(4)], ["cbf", "yrsq"], [pk])
                      r4 = rstd4[:].rearrange("p a t -> p (a t)")
                      ACT(r4, pf[:], AF.Ln, [pk], ["rstd4"], bias=EPS, scale=1.0 / 64)
                      ACT(r4, r4, AF.Exp, ["rstd4"], ["rstd4"], scale=-0.5)
                      TT("dve", yr1[:], yr1[:], bc(ppc("retnw", 0, 4).unsqueeze(2), [P, 4, 128]), ALU.mult, ["yr1", "pp"], ["yr1"])
                      TT("dve", yr1[:], yr1[:], rstd4[:], ALU.mult, ["yr1", "rstd4"], ["yr1"])
                      TT("dve", yTret[:, :, cs_], yr1[:], rg[:, :, cs_], ALU.mult, ["yr1", "rg"], ["yTret"])
                      if debug and s == 0 and l == 0:
                          TT("dve", dbt[:, 0:4, :], yr1[:], rg[:, :, cs_], ALU.mult, ["yr1", "rg"], ["tB"])
                          A.dma("sp", "dbg", dbg["yret"][:, :, cs_], dbt[:, 0:4, :], reads=["tB"], writes=["dbg_out"])

                  stg(5)
                  acc = {(0, 0): (pC[:, 0:512], "pC"), (0, 1): (pC[:, 512:1024], "pC"), (1, 0): (pY[:, 0:512], "pY"), (1, 1): (pY[:, 512:1024], "pY")}
                  for go in range(8):
                      si = (NG + go) % NSLOT
                      wk = "ws%d" % si
                      wv = wslot[si][:].rearrange("p a b -> p (a b)").rearrange("p (a b) -> p a b", a=2)
                      A.dma("sp", wk, wv, woscr_d[go], reads=["woscr%d" % go], writes=[wk])
                      fns = []
                      for c in range(NCH):
                          cs_ = slice(c * T, (c + 1) * T)
                          for half in range(2):
                              for k2 in range(2):
                                  kt = 2 * go + k2
                                  if kt < 8:
                                      lt = yTssd[:, kt, cs_]
                                  elif kt < 12:
                                      lt = yTs5[:, kt - 8, cs_]
                                  else:
                                      lt = yTret[:, kt - 12, cs_]
                                  fns.append(lambda g, c=c, half=half, k2=k2, kt=kt, lt=lt: g.matmul(
                                      acc[(c, half)][0], lt, wv[:, k2, half * 512:(half + 1) * 512], start=(kt == 0), stop=(kt == 15)))
                      A.group("pe", fns, ["yTssd", "yTs5", "yTret", wk], ["pC", "pY"])
                  def epilogue(t0_=t0):
                      for c in range(NCH):
                          rows = slice(t0_ + c * T, t0_ + (c + 1) * T)
                          pacc, pkey = (pC, "pC") if c == 0 else (pY, "pY")
                          A.dma("sp", "hnld", hn[:], src_d[rows, :], writes=["hn"])
                          TT("dve", hn[:], hn[:], pacc[:], ALU.add, ["hn", pkey], ["hn"])
                          if not last:
                              A.dma("sp", "st", hres_d[rows, :], hn[:], reads=["hn"], writes=["hres"])
                          else:
                              ACT(xn[:], hn[:], AF.Square, ["hn"], ["xn", "small"], accum=small[:, 4:5])
                              ACT(small[:, 5:6], small[:, 4:5], AF.Ln, ["small"], ["small"], bias=EPS, scale=1.0 / DM)
                              ACT(small[:, 6:7], small[:, 5:6], AF.Exp, ["small"], ["small"], scale=-0.5)
                              STT(hn[:], hn[:], small[:, 6:7], fnw[:], ALU.mult, ALU.mult, ["hn", "small", "fnw"], ["hn"])
                              A.dma("sp", "st", out_d[rows, :], hn[:], reads=["hn"], writes=["outd"])

                  pend[0] = epilogue
                  CP("dve", xbcT[:, :, 0:3], xbcT[:, :, SP:SP + 3], ["xbcT"], ["xbcT"])
              if pend[0] is not None:
                  pend[0]()
                  pend[0] = None
              A.barrier()
              es3.close()
              open_stacks[:] = []

        except _Stop:
            for st_ in reversed(open_stacks):
                st_.close()
        A.finish("sp")
        build_program.last_nins = A.nins
    return nc


def make_consts():
    c32 = np.zeros((P, NC32), np.float64)
    k = np.arange(128)
    c32[:, C32["U"]:C32["U"] + 128] = (k[:, None] > k[None, :])
    c32[:, C32["ones"]:C32["ones"] + 128] = 1.0
    c32[:, C32["tri"]:C32["tri"] + 128] = (k[:, None] <= k[None, :])
    log_g = np.log1p(-np.exp2(-5.0 - np.arange(8)))
    scale = 32 ** -0.5
    diff = k[None, :] - k[:, None]
    for h in range(8):
        c32[:, C32["dmat"] + h * 128:C32["dmat"] + (h + 1) * 128] = np.where(diff >= 0, np.exp(np.maximum(diff, 0) * log_g[h]) * scale, 0.0)
    for tl in range(2):
        for h4 in range(4):
            h = 4 * tl + h4
            c32[32 * h4:32 * h4 + 32, C32["qdec"] + tl * 128:C32["qdec"] + (tl + 1) * 128] = np.exp((k + 1.0) * log_g[h])[None, :]
            c32[32 * h4:32 * h4 + 32, C32["gch"] + tl] = np.exp(128.0 * log_g[h])
    for h in range(8):
        c32[:, C32["kdec"] + h] = np.exp((127.0 - k) * log_g[h]) * scale
    inv_freq = 10000.0 ** (-np.arange(0, 32, 2) / 32.0)
    fr = inv_freq[np.arange(128) % 16]
    c32[:, C32["cos"]:C32["cos"] + 128] = np.cos(fr[:, None] * k[None, :])
    c32[:, C32["sin"]:C32["sin"] + 128] = np.sin(fr[:, None] * k[None, :])
    cidx = np.arange(64)
    c32[:, C32["cc"]:C32["cc"] + 64] = np.cos(fr[:, None] * 128.0 * cidx[None, :])
    c32[:, C32["sc"]:C32["sc"] + 64] = np.sin(fr[:, None] * 128.0 * cidx[None, :])
    for r in range(4):
        c32[32 * r:32 * r + 32, C32["rmask"] + r] = 1.0
    cbf = np.zeros((P, 512), np.float32)
    cbf[:, 0:128] = np.eye(128)
    cbf[:, 128:256] = 1.0
    perm = np.zeros((128, 128), np.float32)
    for m in range(128):
        if m % 32 < 16:
            perm[m + 16, m] = -1.0
        else:
            perm[m - 16, m] = 1.0
    cbf[:, 256:384] = perm
    blk = np.zeros((128, 128), np.float32)
    blk[0:64, 0:64] = 1.0
    blk[64:128, 64:128] = 1.0
    cbf[:, 384:512] = blk
    return c32.astype(np.float32), cbf


def prep_weights(inp):
    f = lambda a: np.asarray(a, dtype=np.float32)
    w_in = f(inp["w_in"])
    win_g = np.zeros((2, NG, P, 8, 256), np.float32)
    for l in range(2):
        wl = w_in[l].reshape(8, 128, 5136)
        for gi in range(18):
            for tt in range(2):
                c0 = FM_ORIG[2 * gi + tt]
                win_g[l, gi, :, :, tt * 128:(tt + 1) * 128] = wl[:, :, c0:c0 + 128].transpose(1, 0, 2)
        for hv in range(2):
            win_g[l, 18 + hv] = wl[:, :, 4112 + hv * 256:4112 + (hv + 1) * 256].transpose(1, 0, 2)
        win_g[l, 20, :, :, 0:16] = wl[:, :, 2560:2576].transpose(1, 0, 2)
    wout_h = f(inp["w_out"]).reshape(2, 16, 128, DM).transpose(0, 2, 1, 3).copy()
    wglu_h = f(inp["s5_w_glu"]).reshape(2, 4, 128, 512).transpose(0, 2, 1, 3).copy()
    pp = np.zeros((2, P, NPP), np.float32)
    pb = np.zeros((2, P, NPB), np.float32)
    for l in range(2):
        def put(name, arr):
            arr = np.asarray(arr, np.float32)
            if name in OFFB:
                pb[l, :, OFFB[name]:OFFB[name] + arr.shape[1]] = arr
            else:
                pp[l, :, OFF[name]:OFF[name] + arr.shape[1]] = arr
        put("normw", f(inp["norm_w"])[l].reshape(8, 128).T)
        cw = f(inp["conv_w"])[l]
        put("convw", cw.reshape(4, 12, 128).transpose(2, 1, 0).reshape(128, 48))
        put("convb", f(inp["conv_b"])[l].reshape(12, 128).T)
        put("dtb", np.broadcast_to(f(inp["dt_bias"])[l][None, :], (128, 16)))
        put("alog", np.broadcast_to(f(inp["a_log"])[l][None, :], (128, 16)))
        put("dssd", np.broadcast_to(f(inp["d_ssd"])[l][None, :], (128, 16)))
        put("ssdnw", f(inp["ssd_norm_w"])[l].reshape(8, 128).T)
        put("s5d", f(inp["s5_d"])[l].reshape(4, 128).T)
        put("bglu", f(inp["s5_b_glu"])[l].reshape(4, 128).T)
        put("retnw", f(inp["ret_norm_w"])[l].reshape(4, 128).T)
        lam_re = f(inp["s5_lambda_re"])[l].reshape(16, 2, 64)
        lam_im = f(inp["s5_lambda_im"])[l].reshape(16, 2, 64)
        put("lamre", lam_re.transpose(1, 2, 0).reshape(128, 16))
        put("lamim", lam_im.transpose(1, 2, 0).reshape(128, 16))
        ls = f(inp["s5_log_step"])[l].reshape(16, 2)
        put("lstep", np.broadcast_to(ls.T[:, None, :], (2, 64, 16)).reshape(128, 16))
        for nm, src, tr in (("bre", "s5_b_re", False), ("bim", "s5_b_im", False), ("cre", "s5_c_re", True), ("cim", "s5_c_im", True)):
            a = f(inp[src])[l]
            if tr:
                a = a.transpose(0, 2, 1)
            a = a.reshape(16, 2, 64, 16)
            o = np.zeros((2, 64, 16, 2, 16), np.float32)
            for par in range(2):
                o[par, :, :, par, :] = a[:, par].transpose(1, 0, 2)
            put(nm, o.reshape(128, 512))
    cbrow = f(inp["conv_b"])[:, None, :1280].copy()
    fnw = f(inp["final_norm_w"])[None, :].copy()
    c32, cbf = make_consts()
    return {"win_g": win_g, "wout_h": wout_h, "wglu_h": wglu_h, "pp": pp, "pb": pb, "cbrow": cbrow, "fnw": fnw,
            "cst32": c32, "cstbf": cbf}


_prog_cache = {}


def kernel(**inputs):
    x = np.asarray(inputs["x"], dtype=np.float32)
    B, Lx, _ = x.shape
    nspan = Lx // SP
    shared = prep_weights(inputs)
    key = (nspan, 2)
    if key not in _prog_cache:
        _prog_cache[key] = build_program(nspan=nspan, nlayer=2)
    nc = _prog_cache[key]
    ncores = 8
    in_maps = []
    for cidx in range(ncores):
        m = dict(shared)
        m["x"] = np.ascontiguousarray(x[cidx % B])
        in_maps.append(m)
    res = run_bass_kernel_spmd(nc, in_maps, core_ids=list(range(ncores)))
    out = np.stack([np.asarray(res.results[b]["out"], dtype=np.float32) for b in range(B)], axis=0)
    return out.astype(inputs["x"].dtype)
```

```python
import math
import numpy as np
from contextlib import ExitStack
import concourse.bass as bass
import concourse.mybir as mybir
from concourse.bass_utils import run_bass_kernel_spmd

F32 = mybir.dt.float32
BF16 = mybir.dt.bfloat16
AF = mybir.ActivationFunctionType
ALU = mybir.AluOpType

P = 128
T = 128
NCH = 2
SP = T * NCH
J = 4
NB = SP // J
DM = 1024
EPS = 1e-6
NG = 21
SEQ = 8192
TWO_PI = 2.0 * math.pi

FM_ORIG = ([0 + 128 * i for i in range(8)] + [1024 + 128 * i for i in range(12)] +
           [2576 + 128 * i for i in range(4)] + [3088 + 128 * i for i in range(4)] +
           [3600, 3728] + [3856, 3984] + [4624 + 128 * i for i in range(4)])
assert len(FM_ORIG) == 36

_pp_fields = [("normw", 8), ("convw", 48), ("convb", 12), ("dtb", 16), ("alog", 16), ("dssd", 16),
              ("ssdnw", 8), ("s5d", 4), ("bglu", 4), ("retnw", 4), ("lamre", 16), ("lamim", 16),
              ("lstep", 16)]
_pb_fields = [("bre", 512), ("bim", 512), ("cre", 512), ("cim", 512)]
OFF = {}
_o = 0
for _n, _w in _pp_fields:
    OFF[_n] = _o
    _o += _w
NPP = _o
OFFB = {}
_o = 0
for _n, _w in _pb_fields:
    OFFB[_n] = _o
    _o += _w
NPB = _o

_c32_fields = [("U", 128), ("ones", 128), ("tri", 128), ("dmat", 1024), ("qdec", 256), ("kdec", 8),
               ("gch", 2), ("cos", 128), ("sin", 128), ("cc", 64), ("sc", 64), ("rmask", 4)]
C32 = {}
_o = 0
for _n, _w in _c32_fields:
    C32[_n] = _o
    _o += _w
NC32 = _o


class AS:
    def __init__(self, nc, es):
        self.nc = nc
        self.es = es
        self.eng = {"pe": nc.tensor, "act": nc.scalar, "dve": nc.vector, "pool": nc.gpsimd, "sp": nc.sync}
        self.sem = {k: es.enter_context(nc.semaphore("sem_" + k)) for k in self.eng}
        self.cnt = {k: 0 for k in self.eng}
        self.waited = {k: {} for k in self.eng}
        self.lastw = {}
        self.readers = {}
        self.dsem = {}
        self.nins = 0

    def _need(self, e, deps):
        best = {}
        for (src, val) in deps:
            if self.waited[e].get(src, 0) >= val:
                continue
            if best.get(src, 0) < val:
                best[src] = val
        for src, val in best.items():
            sem = self.dsem[src][0] if src in self.dsem else self.sem[src]
            self.eng[e].wait_ge(sem, val)
            self.waited[e][src] = val
            self.nins += 1

    def _deps(self, reads, writes):
        deps = []
        for k in reads:
            w = self.lastw.get(k)
            if w is not None:
                deps.append(w)
        for k in writes:
            w = self.lastw.get(k)
            if w is not None:
                deps.append(w)
            deps.extend(self.readers.get(k, ()))
        return deps

    def _commit(self, tag, reads, writes):
        for k in reads:
            lst = self.readers.setdefault(k, [])
            lst[:] = [t for t in lst if t[0] != tag[0]]
            lst.append(tag)
        for k in writes:
            self.lastw[k] = tag
            self.readers[k] = []

    def op(self, e, fn, reads=(), writes=()):
        self._need(e, self._deps(reads, writes))
        ins = fn(self.eng[e])
        self.cnt[e] += 1
        ins.then_inc(self.sem[e], 1)
        self.nins += 1
        self._commit((e, self.cnt[e]), reads, writes)

    def group(self, e, fns, reads=(), writes=()):
        self._need(e, self._deps(reads, writes))
        ins = None
        for fn in fns:
            ins = fn(self.eng[e])
            self.nins += 1
        self.cnt[e] += 1
        ins.then_inc(self.sem[e], 1)
        self._commit((e, self.cnt[e]), reads, writes)

    def dma(self, q, chan, out, in_, reads=(), writes=(), **kw):
        if chan not in self.dsem:
            self.dsem[chan] = [self.es.enter_context(self.nc.semaphore("dsem_" + chan)), 0]
        self._need(q, self._deps(reads, writes))
        if q == "pool":
            kw.setdefault("max_dma_last_dim", 2048)
        ins = self.eng[q].dma_start(out=out, in_=in_, **kw)
        self.dsem[chan][1] += 16
        ins.then_inc(self.dsem[chan][0], 16)
        self.nins += 1
        self._commit((chan, self.dsem[chan][1]), reads, writes)

    def barrier(self):
        for e in self.eng:
            deps = [(s_, self.cnt[s_]) for s_ in self.eng if s_ != e and self.cnt[s_] > 0]
            deps += [(ch, v[1]) for ch, v in self.dsem.items() if v[1] > 0]
            self._need(e, deps)

    def finish(self, e="sp"):
        deps = list(self.lastw.values())
        for l in self.readers.values():
            deps.extend(l)
        self._need(e, deps)


import os
VAR_RMAX = int(os.environ.get("K_RMAX", "4"))
VAR_SKIPROT = int(os.environ.get("K_SKIPROT", "0"))


class _Stop(Exception):
    pass


def build_program(nspan=SEQ // SP, nlayer=2, debug=False, stage=None):
    nc = bass.Bass("TRN2", target_bir_lowering=False)

    def stg(k):
        if stage == k:
            raise _Stop()
    L = nspan * SP
    dram = lambda n, s, d, k: nc.dram_tensor(n, s, d, kind=k).ap()
    x_d = dram("x", [L, DM], F32, "ExternalInput")
    win_d = dram("win_g", [2, NG, P, 8, 256], F32, "ExternalInput")
    wout_d = dram("wout_h", [2, P, 16, DM], F32, "ExternalInput")
    wglu_d = dram("wglu_h", [2, P, 4, 512], F32, "ExternalInput")
    pp_d = dram("pp", [2, P, NPP], F32, "ExternalInput")
    pb_d = dram("pb", [2, P, NPB], F32, "ExternalInput")
    cbrow_d = dram("cbrow", [2, 1, 1280], F32, "ExternalInput")
    fnw_d = dram("fnw", [1, DM], F32, "ExternalInput")
    c32_d = dram("cst32", [P, NC32], F32, "ExternalInput")
    cbf_d = dram("cstbf", [P, 512], F32, "ExternalInput")
    out_d = dram("out", [L, DM], F32, "ExternalOutput")
    hres_d = dram("hres", [L, DM], F32, "Internal")
    wscr_d = dram("wscr", [NG, P, 8, 256], BF16, "Internal")
    woscr_d = dram("woscr", [8, P, 2, DM], BF16, "Internal")
    dbg = {}
    if debug:
        dbg["yssd"] = dram("d_yssd", [P, 8, SP], F32, "ExternalOutput")
        dbg["ys5"] = dram("d_ys5", [P, 4, SP], F32, "ExternalOutput")
        dbg["yret"] = dram("d_yret", [P, 4, SP], F32, "ExternalOutput")

    with ExitStack() as es:
        A = AS(nc, es)
        sb = lambda n, s, d: es.enter_context(nc.sbuf_tensor("s_" + n, s, d))
        pst = lambda n, s, d: es.enter_context(nc.psum_tensor("p_" + n, s, d))

        pT = pst("pT", [P, 1024], BF16)
        pF = [pst("pF0", [P, 512], F32), pst("pF1", [P, 512], F32)]
        pS = pst("pS", [P, 512], F32)
        pY = pst("pY", [P, 1024], F32)
        pC = pst("pC", [P, 1024], F32)
        pTf = pT.bitcast(F32)
        pfi = [0]

        def nextF():
            pfi[0] ^= 1
            return pF[pfi[0]], "pF%d" % pfi[0]

        c32 = sb("c32", [P, NC32], F32)
        cbf = sb("cbf", [P, 512], BF16)
        identb = cbf[:, 0:128]
        onesb = cbf[:, 128:256]
        permb = cbf[:, 256:384]
        blk64b = cbf[:, 384:512]
        U32 = c32[:, C32["U"]:C32["U"] + 128]
        ones32 = c32[:, C32["ones"]:C32["ones"] + 128]
        tri32 = c32[:, C32["tri"]:C32["tri"] + 128]
        dmat = c32[:, C32["dmat"]:C32["dmat"] + 1024].rearrange("p (h t) -> p h t", h=8)
        qdec = c32[:, C32["qdec"]:C32["qdec"] + 256].rearrange("p (a t) -> p a t", a=2)
        kdec = c32[:, C32["kdec"]:C32["kdec"] + 8]
        gch = c32[:, C32["gch"]:C32["gch"] + 2]
        cos_ti = c32[:, C32["cos"]:C32["cos"] + 128]
        sin_ti = c32[:, C32["sin"]:C32["sin"] + 128]
        cc_t = c32[:, C32["cc"]:C32["cc"] + 64]
        sc_t = c32[:, C32["sc"]:C32["sc"] + 64]
        rmask = c32[:, C32["rmask"]:C32["rmask"] + 4]
        Gb = [(pF[0], "pF0"), (pF[1], "pF1"), (pS, "pS"), (pC, "pC")]

        pp = sb("pp", [P, NPP], F32)
        ppc = lambda name, i=0, n=1: pp[:, OFF[name] + i:OFF[name] + i + n]
        cbrow = sb("cbrow", [1, 1280], BF16)
        fnw = sb("fnw", [P, DM], F32)
        wglu = sb("wglu", [P, 4, 512], BF16)
        NSLOT = 4
        wslot = [sb("wslot%d" % i, [P, 8, 256], BF16) for i in range(NSLOT)]
        diagw = sb("diagw", [P, 12, 4, 128], BF16)
        dI = sb("dI", [P, 16, 128], BF16)
        Ab = sb("Ab", [P, 16], F32)
        hbglu = sb("hbglu", [P, 4], F32)
        WG = sb("WG", [P, 4, J, 2, 128], BF16)
        Cl = sb("Cl", [P, 16, J, 2, 32], BF16)
        Ktap = sb("Ktap", [P, 4, J, 128], BF16)
        phc = sb("phc", [P, 16, NB + 1], F32)
        phs = sb("phs", [P, 16, NB + 1], F32)
        Rr = sb("Rr", [P, 16], F32)
        prevT = sb("prevT", [P, 1024], F32)
        prevTb = sb("prevTb", [P, 1024], BF16)
        rstate = sb("rstate", [P, 2, 64], F32)
        rstateb = sb("rstateb", [P, 2, 64], BF16)
        rspad = sb("rspad", [P, 8, 64], BF16)
        Vre = sb("Vre", [P, 16, NB + 1], F32)
        Vim = sb("Vim", [P, 16, NB + 1], F32)
        small = sb("small", [P, 16], F32)
        def TT(e, out, a, b, op, r, w):
            A.op(e, lambda g: g.tensor_tensor(out, a, b, op), r, w)

        def TS(e, out, a, s1, op0, r, w, s2=None, op1=None):
            if op1 is None:
                A.op(e, lambda g: g.tensor_scalar(out, a, s1, None, op0), r, w)
            else:
                A.op(e, lambda g: g.tensor_scalar(out, a, s1, s2, op0, op1), r, w)

        def STT(out, a, s, b, op0, op1, r, w):
            A.op("dve", lambda g: g.scalar_tensor_tensor(out, a, s, b, op0, op1), r, w)

        def ACT(out, a, func, r, w, bias=None, scale=None, accum=None):
            kw = {}
            if bias is not None:
                kw["bias"] = bias
            if scale is not None:
                kw["scale"] = scale
            if accum is not None:
                kw["accum_out"] = accum
            A.op("act", lambda g: g.activation(out, a, func, **kw), r, w)

        def CP(e, out, a, r, w):
            A.op(e, lambda g: g.tensor_copy(out, a), r, w)

        def bc(ap, shape):
            return ap.broadcast_to(shape)

        A.dma("sp", "cst", c32[:], c32_d, writes=["c32"])
        A.dma("pool", "cstb", cbf[:], cbf_d, writes=["cbf"])
        A.dma("sp", "cst", fnw[:], fnw_d.partition_broadcast(P), writes=["fnw"])

        open_stacks = []
        try:
          for l in range(nlayer):
              src_d = x_d if l == 0 else hres_d
              last = (l == nlayer - 1)
              A.barrier()
              es2 = ExitStack()
              open_stacks[:] = [es2]
              sb2 = lambda n, s_, d: es2.enter_context(nc.sbuf_tensor("s_%s_L%d" % (n, l), s_, d))
              pb = sb2("pb", [P, NPB], F32)
              pbc = lambda name: pb[:, OFFB[name]:OFFB[name] + 512]
              A.dma("sp", "pb", pb[:], pb_d[l], writes=["s5p"])
              A.dma("sp", "pp", pp[:], pp_d[l], writes=["pp"])
              A.dma("pool", "cbrow", cbrow[:], cbrow_d[l], writes=["cbrow"])
              A.dma("pool", "wglu", wglu[:], wglu_d[l], writes=["wglu"])
              stg(0.1)
              for gi in range(NG):
                  si = gi % NSLOT
                  A.dma("pool", "wsq%d" % si, wslot[si][:], win_d[l, gi], writes=["ws%d" % si])
                  A.dma("sp", "wscr%d" % gi, wscr_d[gi], wslot[si][:], reads=["ws%d" % si], writes=["wscr%d" % gi])
              for go in range(8):
                  si = (NG + go) % NSLOT
                  wv = wslot[si][:].rearrange("p a b -> p (a b)").rearrange("p (a b) -> p a b", a=2)
                  A.dma("pool", "wsq%d" % si, wv, wout_d[l][:, 2 * go:2 * go + 2, :], writes=["ws%d" % si])
                  A.dma("sp", "woscr%d" % go, woscr_d[go], wv, reads=["ws%d" % si], writes=["woscr%d" % go])
              stg(0.2)
              for tl in range(12):
                  for k in range(4):
                      TS("pool", diagw[:, tl, k, :], identb, ppc("convw", tl * 4 + k), ALU.mult, ["cbf", "pp"], ["diagw"])
              for h in range(16):
                  TS("pool", dI[:, h, :], identb, ppc("dssd", h), ALU.mult, ["cbf", "pp"], ["dI"])
              ACT(Ab[:], ppc("alog", 0, 16), AF.Exp, ["pp"], ["Ab"])
              TS("dve", Ab[:], Ab[:], -1.0, ALU.mult, ["Ab"], ["Ab"])
              TS("dve", hbglu[:], ppc("bglu", 0, 4), 0.5, ALU.mult, ["pp"], ["hbglu"])

              stg(0.3)
              s16 = lambda n: sb2("s5_%s" % n, [P, 16], F32)
              step, lrs, ang, mag, sn, cs, angc, msk = [s16(n) for n in ("step", "lrs", "ang", "mag", "sn", "cs", "angc", "msk")]
              lbr, lbi, den, aa, fre, fim, t16a, t16b = [s16(n) for n in ("lbr", "lbi", "den", "aa", "fre", "fim", "t16a", "t16b")]
              K5 = ["s5p"]

              def e16(e, out, a, b, op):
                  TT(e, out, a, b, op, K5, K5)

              ACT(step[:], ppc("lstep", 0, 16), AF.Exp, ["pp"], K5)
              e16("dve", lrs[:], ppc("lamre", 0, 16), step[:], ALU.mult)
              e16("dve", ang[:], ppc("lamim", 0, 16), step[:], ALU.mult)
              ACT(mag[:], lrs[:], AF.Exp, K5, K5)
              ACT(Rr[:], lrs[:], AF.Exp, K5, K5 + ["Rr"], scale=float(J))
              for _ in range(5):
                  TS("dve", msk[:], ang[:], math.pi, ALU.is_gt, K5, K5, s2=TWO_PI, op1=ALU.mult)
                  e16("dve", ang[:], ang[:], msk[:], ALU.subtract)
              TS("dve", angc[:], ang[:], math.pi / 2, ALU.add, K5, K5)
              TS("dve", msk[:], angc[:], math.pi, ALU.is_gt, K5, K5, s2=TWO_PI, op1=ALU.mult)
              e16("dve", angc[:], angc[:], msk[:], ALU.subtract)
              ACT(sn[:], ang[:], AF.Sin, K5, K5)
              ACT(cs[:], angc[:], AF.Sin, K5, K5)
              e16("dve", lbr[:], mag[:], cs[:], ALU.mult)
              e16("dve", lbi[:], mag[:], sn[:], ALU.mult)
              e16("dve", den[:], ppc("lamre", 0, 16), ppc("lamre", 0, 16), ALU.mult)
              e16("dve", t16a[:], ppc("lamim", 0, 16), ppc("lamim", 0, 16), ALU.mult)
              e16("dve", den[:], den[:], t16a[:], ALU.add)
              A.op("dve", lambda g: g.reciprocal(den[:], den[:]), K5, K5)
              TS("dve", aa[:], lbr[:], -1.0, ALU.add, K5, K5)
              e16("dve", t16a[:], aa[:], ppc("lamre", 0, 16), ALU.mult)
              e16("dve", t16b[:], lbi[:], ppc("lamim", 0, 16), ALU.mult)
              e16("dve", fre[:], t16a[:], t16b[:], ALU.add)
              e16("dve", fre[:], fre[:], den[:], ALU.mult)
              e16("dve", t16a[:], lbi[:], ppc("lamre", 0, 16), ALU.mult)
              e16("dve", t16b[:], aa[:], ppc("lamim", 0, 16), ALU.mult)
              e16("dve", fim[:], t16a[:], t16b[:], ALU.subtract)
              e16("dve", fim[:], fim[:], den[:], ALU.mult)
              s512 = lambda n: sb2("s5_%s" % n, [P, 16, 32], F32)
              bbr, bbi, t5a, t5b, ncim = [s512(n) for n in ("bbr", "bbi", "t5a", "t5b", "ncim")]
              b_re = pbc("bre").rearrange("p (a b) -> p a b", a=16)
              b_im = pbc("bim").rearrange("p (a b) -> p a b", a=16)
              c_re = pbc("cre").rearrange("p (a b) -> p a b", a=16)
              c_im = pbc("cim").rearrange("p (a b) -> p a b", a=16)
              b32 = lambda t: bc(t[:].unsqueeze(2), [P, 16, 32])

              def cmul(o_r, o_i, ar, ai, br, bi, sh_b):
                  TT("dve", t5a[:], ar, sh_b(br), ALU.mult, K5, K5)
                  TT("dve", t5b[:], ai, sh_b(bi), ALU.mult, K5, K5)
                  TT("dve", o_r, t5a[:], t5b[:], ALU.subtract, K5, K5)
                  TT("dve", t5a[:], ar, sh_b(bi), ALU.mult, K5, K5)
                  TT("dve", t5b[:], ai, sh_b(br), ALU.mult, K5, K5)
                  TT("dve", o_i, t5a[:], t5b[:], ALU.add, K5, K5)

              cmul(bbr[:], bbi[:], b_re, b_im, fre, fim, b32)
              TS("dve", ncim[:], c_im, -1.0, ALU.mult, K5, K5)
              Xr = [bbr] + [s512("xr%d" % k) for k in range(1, J)]
              Xi = [bbi] + [s512("xi%d" % k) for k in range(1, J)]
              for k in range(1, J):
                  cmul(Xr[k][:], Xi[k][:], Xr[k - 1][:], Xi[k - 1][:], lbr, lbi, b32)
              clr_prev, cli_prev = c_re, c_im
              clr = [s512("clr%d" % k) for k in range(J)]
              cli = [s512("cli%d" % k) for k in range(J)]
              for ti in range(J):
                  cmul(clr[ti][:], cli[ti][:], clr_prev, cli_prev, lbr, lbi, b32)
                  clr_prev, cli_prev = clr[ti][:], cli[ti][:]
                  CP("dve", Cl[:, :, ti, 0, :], clr[ti][:], K5, ["Cl"])
                  TS("dve", Cl[:, :, ti, 1, :], cli[ti][:], -1.0, ALU.mult, K5, ["Cl"])
              Xrb = [sb2("s5_xrb%d" % k, [P, 16, 32], BF16) for k in range(J)]
              Xib = [sb2("s5_xib%d" % k, [P, 16, 32], BF16) for k in range(J)]
              for k in range(J):
                  CP("dve", Xrb[k][:], Xr[k][:], K5, K5)
                  CP("dve", Xib[k][:], Xi[k][:], K5, K5)
              stg(0.4)
              for pair in range(16):
                  q, r = pair // 4, pair % 4
                  for ti in range(J):
                      for part, Xb in ((0, Xrb), (1, Xib)):
                          pf, pk = nextF()
                          A.op("pe", lambda g: g.matmul(pf[32 * r:32 * r + 32, 0:128], Xb[J - 1 - ti][:, pair, :], identb,
                                                         start=True, stop=True, tile_position=(0, 32 * r)),
                               K5 + ["cbf"], [pk])
                          CP("dve", WG[32 * r:32 * r + 32, q, ti, part, :], pf[32 * r:32 * r + 32, 0:128], [pk], ["WG"])
              A.op("pool", lambda g: g.memset(Ktap[:], 0.0), [], ["Ktap"])
              for q in range(4):
                  for j in range(J):
                      pf, pk = nextF()
                      A.group("pe", [
                          lambda g: g.matmul(pf[:, 0:128], Xr[j][:, 4 * q:4 * q + 4, :].rearrange("p a b -> p (a b)"),
                                             c_re[:, 4 * q:4 * q + 4, :].rearrange("p a b -> p (a b)"), start=True, stop=False),
                          lambda g: g.matmul(pf[:, 0:128], Xi[j][:, 4 * q:4 * q + 4, :].rearrange("p a b -> p (a b)"),
                                             ncim[:, 4 * q:4 * q + 4, :].rearrange("p a b -> p (a b)"), start=False, stop=True),
                      ], K5, [pk])
                      for r in range(4):
                          CP("dve", Ktap[32 * r:32 * r + 32, q, j, 32 * r:32 * r + 32], pf[32 * r:32 * r + 32, 32 * r:32 * r + 32], [pk], ["Ktap"])
              eJr, eJi = s16("eJr"), s16("eJi")
              CP("dve", eJr[:], cs[:], K5, K5)
              CP("dve", eJi[:], sn[:], K5, K5)
              for _ in range(J - 1):
                  e16("dve", t16a[:], eJr[:], cs[:], ALU.mult)
                  e16("dve", t16b[:], eJi[:], sn[:], ALU.mult)
                  e16("dve", aa[:], t16a[:], t16b[:], ALU.subtract)
                  e16("dve", t16a[:], eJr[:], sn[:], ALU.mult)
                  e16("dve", t16b[:], eJi[:], cs[:], ALU.mult)
                  e16("dve", eJi[:], t16a[:], t16b[:], ALU.add)
                  CP("dve", eJr[:], aa[:], K5, K5)
              KP = ["ph"]
              A.op("dve", lambda g: g.memset(phc[:, :, 0:1], 1.0), [], KP)
              A.op("dve", lambda g: g.memset(phs[:, :, 0:1], 0.0), [], KP)
              CP("dve", phc[:, :, 1], eJr[:], K5, KP)
              CP("dve", phs[:, :, 1], eJi[:], K5, KP)
              tph = sb2("s5_tph", [P, 16, 32], F32)
              tph2 = sb2("s5_tph2", [P, 16, 32], F32)
              n = 1
              while n < NB:
                  cn = bc(phc[:, :, n:n + 1], [P, 16, n])
                  sn_ = bc(phs[:, :, n:n + 1], [P, 16, n])
                  TT("dve", tph[:, :, 0:n], phc[:, :, 1:n + 1], cn, ALU.mult, KP, KP)
                  TT("dve", tph2[:, :, 0:n], phs[:, :, 1:n + 1], sn_, ALU.mult, KP, KP)
                  TT("dve", phc[:, :, n + 1:2 * n + 1], tph[:, :, 0:n], tph2[:, :, 0:n], ALU.subtract, KP, KP)
                  TT("dve", tph[:, :, 0:n], phc[:, :, 1:n + 1], sn_, ALU.mult, KP, KP)
                  TT("dve", tph2[:, :, 0:n], phs[:, :, 1:n + 1], cn, ALU.mult, KP, KP)
                  TT("dve", phs[:, :, n + 1:2 * n + 1], tph[:, :, 0:n], tph2[:, :, 0:n], ALU.add, KP, KP)
                  n *= 2

              stg(1)
              A.barrier()
              es2.close()
              es3 = ExitStack()
              open_stacks[:] = [es3]
              sb3 = lambda n, s_, d: es3.enter_context(nc.sbuf_tensor("s_%s_L%d" % (n, l), s_, d))
              xt = [sb3("xt0", [P, DM], F32)] * 2
              xn = sb3("xn", [P, DM], BF16)
              hTs = [sb3("hT0", [P, 8, SP], BF16), sb3("hT1", [P, 8, SP], BF16)]
              zs = sb3("zs", [P, 8, SP], BF16)
              xbcT = sb3("xbcT", [P, 12, 3 + SP], BF16)
              g5 = sb3("g5", [P, 4, SP], BF16)
              uT = sb3("uT", [P, 4, SP], BF16)
              qT = sb3("qT", [P, 2, SP], BF16)
              kT = sb3("kT", [P, 2, SP], BF16)
              rg = sb3("rg", [P, 4, SP], BF16)
              vtok = [sb3("vtok%d" % i, [P, 512], BF16) for i in range(NCH)]
              dtt = [sb3("dt%d" % i, [P, 16], F32) for i in range(NCH)]
              dtA = [sb3("dtA%d" % i, [P, 16], F32) for i in range(NCH)]
              BCT = sb3("BCT", [P, 4, SP], BF16)
              yTs5 = sb3("yTs5", [P, 4, SP], BF16)
              cre = sb3("cre", [P, 16, NB], F32)
              cim = sb3("cim", [P, 16, NB], F32)
              Sre = sb3("Sre", [P, 16, NB], BF16)
              Sim = sb3("Sim", [P, 16, NB], BF16)
              tA = sb3("tA", [P, 1024], F32)
              tB = sb3("tB", [P, 1024], F32)
              y5a = sb3("y5a", [P, 4, SP], F32)
              y5b = sb3("y5b", [P, 4, SP], BF16)
              xstok = sb3("xstok", [P, 1024], BF16)
              xsw = sb3("xsw", [P, 1024], BF16)
              Btok = sb3("Btok", [P, 256], BF16)
              cbTm = sb3("cbTm", [P, 2, 128], F32)
              Dq_2 = [sb3("Dq0", [P, 4, 128], F32)] * 2
              dec_2 = [sb3("dec0", [P, 4, 128], F32), sb3("dec1", [P, 4, 128], F32)]
              eac_2 = [sb3("eac0", [P, 4, 128], F32), sb3("eac1", [P, 4, 128], F32)]
              Mq_2 = [sb3("Mq0", [P, 4, 128], BF16), sb3("Mq1", [P, 4, 128], BF16)]
              CTs_2 = [sb3("CTs0", [P, 4, 128], BF16), sb3("CTs1", [P, 4, 128], BF16)]
              y1 = sb3("y1", [P, 8, 128], F32)
              ysq = sb3("ysq", [P, 8, 128], BF16)
              rstd2 = sb3("rstd2", [P, 2, 128], F32)
              yTssd = sb3("yTssd", [P, 8, SP], BF16)
              wls = sb3("wls", [P, 16], F32)
              cosT = sb3("cosT", [P, 128], F32)
              sinT = sb3("sinT", [P, 128], F32)
              qb = sb3("qb", [P, 2, 128], BF16)
              qdb = sb3("qdb", [P, 2, 128], BF16)
              kb = sb3("kb", [P, 2, 128], BF16)
              kdtok = sb3("kdtok", [P, 256], BF16)
              ST = sb3("ST", [P, 8, 128], BF16)
              yr1 = sb3("yr1", [P, 4, 128], F32)
              yrsq = sb3("yrsq", [P, 4, 128], BF16)
              rstd4 = sb3("rstd4", [P, 4, 128], F32)
              yTret = sb3("yTret", [P, 4, SP], BF16)
              hn = sb3("hn", [P, DM], F32)
              dbt = tB[:].rearrange("p (a t) -> p a t", a=8)
              A.op("pool", lambda g: g.memset(prevT[:], 0.0), [], ["prevT"])
              A.op("pool", lambda g: g.memset(prevTb[:], 0.0), [], ["prevTb"])
              A.op("pool", lambda g: g.memset(rstate[:], 0.0), [], ["rstate"])
              A.op("pool", lambda g: g.memset(rstateb[:], 0.0), [], ["rstateb"])
              A.op("pool", lambda g: g.memset(rspad[:], 0.0), [], ["rspad"])
              A.op("pool", lambda g: g.memset(Vre[:], 0.0), [], ["Vre"])
              A.op("pool", lambda g: g.memset(Vim[:], 0.0), [], ["Vim"])
              A.op("pool", lambda g: g.memset(xbcT[:, :, 0:3], 0.0), [], ["xbcT"])

              def front_end(s):
                  t0 = s * SP
                  hT = hTs[s % 2]
                  hk = "hT%d" % (s % 2)
                  for c in range(NCH):
                      xk = "xt0"
                      A.dma("sp", xk, xt[c][:], src_d[t0 + c * T:t0 + (c + 1) * T, :], writes=[xk])
                      ACT(xn[:], xt[c][:], AF.Square, [xk], ["xn", "small"], accum=small[:, 0:1])
                      ACT(small[:, 1:2], small[:, 0:1], AF.Ln, ["small"], ["small"], bias=EPS, scale=1.0 / DM)
                      ACT(small[:, 2:3], small[:, 1:2], AF.Exp, ["small"], ["small"], scale=-0.5)
                      ACT(xn[:], xt[c][:], AF.Copy, [xk, "small"], ["xn"], scale=small[:, 2:3])
                      A.group("pe", [(lambda g, kt=kt: g.transpose(pT[:, kt * 128:(kt + 1) * 128], xn[:, kt * 128:(kt + 1) * 128], identb))
                                     for kt in range(8)], ["xn", "cbf"], ["pT"])
                      TT("dve", hT[:, :, c * T:(c + 1) * T], pT[:].rearrange("p (k t) -> p k t", k=8),
                         bc(ppc("normw", 0, 8).unsqueeze(2), [P, 8, T]), ALU.mult, ["pT", "pp"], [hk])

              front_end(0)
              for s in range(nspan):
                  t0 = s * SP
                  hT = hTs[s % 2]
                  hk = "hT%d" % (s % 2)
                  Gs5 = [(pS, "pS"), (pC[:, 0:512], "pC"), (pC[:, 512:1024], "pC"), (pY[:, 0:512], "pY")]
                  def s5_pre():
                      fns = []
                      for bt in range(4):
                          for part in range(2):
                              for ti in range(J):
                                  for r in range(4):
                                      uv = uT[32 * r:32 * r + 32, bt, :].rearrange("p (b j) -> p b j", j=J)
                                      fns.append(lambda g, r=r, part=part, ti=ti, uv=uv, bt=bt: g.matmul(
                                          Gs5[r][0][:, (bt * 2 + part) * NB:(bt * 2 + part + 1) * NB], WG[32 * r:32 * r + 32, bt, ti, part, :],
                                          uv[:, :, ti], start=(ti == 0), stop=(ti == J - 1), tile_position=(32 * r, 0)))
                      A.group("pe", fns, ["WG", "uT"], ["pS", "pC", "pY"])
                      for r in range(4):
                          gk = Gs5[r][1]
                          Gv = Gs5[r][0][:, 0:512].rearrange("p (t a b) -> p t a b", t=4, a=2)
                          tAv = tA[:, 0:4 * NB].rearrange("p (r b) -> p r b", r=4)
                          tBv = tB[:, 0:4 * NB].rearrange("p (r b) -> p r b", r=4)
                          TT("dve", tAv, Gv[:, :, 0, :], phc[:, r:16:4, 1:NB + 1], ALU.mult, [gk, "ph"], ["tA"])
                          TT("dve", tBv, Gv[:, :, 1, :], phs[:, r:16:4, 1:NB + 1], ALU.mult, [gk, "ph"], ["tB"])
                          TT("dve", cre[:, r:16:4, :], tAv, tBv, ALU.add, ["tA", "tB"], ["cre"])
                          TT("dve", tAv, Gv[:, :, 1, :], phc[:, r:16:4, 1:NB + 1], ALU.mult, [gk, "ph"], ["tA"])
                          TT("dve", tBv, Gv[:, :, 0, :], phs[:, r:16:4, 1:NB + 1], ALU.mult, [gk, "ph"], ["tB"])
                          TT("dve", cim[:, r:16:4, :], tAv, tBv, ALU.subtract, ["tA", "tB"], ["cim"])

                  for gi in range(NG):
                      si = gi % NSLOT
                      wk = "ws%d" % si
                      A.dma("sp", wk, wslot[si][:], wscr_d[gi], reads=["wscr%d" % gi], writes=[wk])
                      if gi < 18:
                          for tt in range(2):
                              ft = 2 * gi + tt
                              pf, pk = nextF()
                              A.group("pe", [(lambda g, kt=kt: g.matmul(pf[:, 0:SP], wslot[si][:, kt, tt * 128:(tt + 1) * 128], hT[:, kt, :],
                                                                        start=(kt == 0), stop=(kt == 7))) for kt in range(8)],
                                      [wk, hk], [pk])
                              if ft < 8:
                                  ACT(zs[:, ft, :], pf[:, 0:SP], AF.Silu, [pk], ["zs"])
                              elif ft < 20:
                                  CP("dve", xbcT[:, ft - 8, 3:3 + SP], pf[:, 0:SP], [pk], ["xbcT"])
                              elif ft < 24:
                                  ACT(g5[:, ft - 20, :], pf[:, 0:SP], AF.Silu, [pk], ["g5"])
                              elif ft < 28:
                                  CP("dve", uT[:, ft - 24, :], pf[:, 0:SP], [pk], ["uT"])
                              elif ft < 30:
                                  A.op("act", lambda g: g.copy(qT[:, ft - 28, :], pf[:, 0:SP]), [pk], ["qT"])
                              elif ft < 32:
                                  A.op("act", lambda g: g.copy(kT[:, ft - 30, :], pf[:, 0:SP]), [pk], ["kT"])
                              else:
                                  ACT(rg[:, ft - 32, :], pf[:, 0:SP], AF.Silu, [pk], ["rg"])
                      if gi == 13:
                          s5_pre()
                      if gi < 18:
                          pass
                      elif gi < 20:
                          hv = gi - 18
                          for c in range(NCH):
                              pf, pk = nextF()
                              A.group("pe", [(lambda g, kt=kt: g.matmul(pf[:, 0:256], hT[:, kt, c * T:(c + 1) * T], wslot[si][:, kt, :],
                                                                        start=(kt == 0), stop=(kt == 7))) for kt in range(8)],
                                      [wk, hk], [pk])
                              A.op("act", lambda g: g.copy(vtok[c][:, hv * 256:(hv + 1) * 256], pf[:, 0:256]), [pk], ["vtok%d" % c])
                      else:
                          for c in range(NCH):
                              pf, pk = nextF()
                              A.group("pe", [(lambda g, kt=kt: g.matmul(pf[:, 0:16], hT[:, kt, c * T:(c + 1) * T], wslot[si][:, kt, 0:16],
                                                                        start=(kt == 0), stop=(kt == 7))) for kt in range(8)],
                                      [wk, hk], [pk])
                              dk = "dt%d" % c
                              TT("dve", dtt[c][:], pf[:, 0:16], ppc("dtb", 0, 16), ALU.add, [pk, "pp"], [dk])
                              ACT(dtt[c][:], dtt[c][:], AF.Exp, [dk], [dk])
                              ACT(dtt[c][:], dtt[c][:], AF.Ln, [dk], [dk], bias=1.0)
                              TT("dve", dtA[c][:], dtt[c][:], Ab[:], ALU.mult, [dk, "Ab"], ["dtA%d" % c])

                  if s + 1 < nspan:
                      front_end(s + 1)
                  stg(2)
                  for i4 in range(4):
                      tl = 8 + i4
                      pf, pk = nextF()
                      A.group("pe", [(lambda g, k=k: g.matmul(pf[:, 0:SP], diagw[:, tl, k, :], xbcT[:, tl, k:k + SP],
                                                              start=(k == 0), stop=(k == 3))) for k in range(4)],
                              ["diagw", "xbcT"], [pk])
                      ACT(BCT[:, i4, :], pf[:, 0:SP], AF.Silu, [pk, "pp"], ["BCT"], bias=ppc("convb", tl))

                  stg(2.5)
                  stg(2.6)
                  for pair in range(16):
                      A.op("dve", lambda g: g.tensor_tensor_scan(Vre[:, pair, 1:NB + 1], bc(Rr[:, pair:pair + 1], [P, NB]), cre[:, pair, :],
                                                                 Vre[:, pair, 0:1], ALU.mult, ALU.add), ["Rr", "cre", "Vre"], ["Vre"])
                      A.op("dve", lambda g: g.tensor_tensor_scan(Vim[:, pair, 1:NB + 1], bc(Rr[:, pair:pair + 1], [P, NB]), cim[:, pair, :],
                                                                 Vim[:, pair, 0:1], ALU.mult, ALU.add), ["Rr", "cim", "Vim"], ["Vim"])
                  tA3 = tA[:].rearrange("p (a b) -> p a b", a=16)
                  tB3 = tB[:].rearrange("p (a b) -> p a b", a=16)
                  TT("dve", tA3, Vre[:, :, 0:NB], phc[:, :, 0:NB], ALU.mult, ["Vre", "ph"], ["tA"])
                  TT("dve", tB3, Vim[:, :, 0:NB], phs[:, :, 0:NB], ALU.mult, ["Vim", "ph"], ["tB"])
                  TT("dve", Sre[:], tA3, tB3, ALU.subtract, ["tA", "tB"], ["Sre"])
                  TT("dve", tA3, Vre[:, :, 0:NB], phs[:, :, 0:NB], ALU.mult, ["Vre", "ph"], ["tA"])
                  TT("dve", tB3, Vim[:, :, 0:NB], phc[:, :, 0:NB], ALU.mult, ["Vim", "ph"], ["tB"])
                  TT("dve", Sim[:], tA3, tB3, ALU.add, ["tA", "tB"], ["Sim"])
                  tcr = tA[:, 0:16]
                  tci = tB[:, 0:16]
                  tc2 = tA[:, 16:32]
                  tc3 = tB[:, 16:32]
                  TT("dve", tcr, Vre[:, :, NB], phc[:, :, NB], ALU.mult, ["Vre", "ph", "Sre", "Sim"], ["tA"])
                  TT("dve", tci, Vim[:, :, NB], phs[:, :, NB], ALU.mult, ["Vim", "ph", "Sre", "Sim"], ["tB"])
                  TT("dve", tc2, Vre[:, :, NB], phs[:, :, NB], ALU.mult, ["Vre", "ph"], ["tA"])
                  TT("dve", tc3, Vim[:, :, NB], phc[:, :, NB], ALU.mult, ["Vim", "ph"], ["tB"])
                  TT("dve", Vre[:, :, 0], tcr, tci, ALU.subtract, ["tA", "tB", "Sre", "Sim"], ["Vre"])
                  TT("dve", Vim[:, :, 0], tc2, tc3, ALU.add, ["tA", "tB", "Sre", "Sim"], ["Vim"])
                  stg(2.7)
                  for q in range(4):
                      pf, pk = nextF()
                      ov = pf[:, 0:SP].rearrange("p (b j) -> p b j", j=J)
                      uvq = uT[:, q, :].rearrange("p (b j) -> p b j", j=J)
                      fns = []
                      fns.append(lambda g: g.matmul(pf[:, 0:SP], Ktap[:, q, 0, :], uT[:, q, :], start=True, stop=True))
                      for j in range(1, J):
                          for ti in range(j, J):
                              fns.append(lambda g, j=j, ti=ti: g.matmul(ov[:, :, ti], Ktap[:, q, j, :], uvq[:, :, ti - j],
                                                                        start=False, stop=True, skip_group_check=True))
                      for r in range(4):
                          pair = 4 * q + r
                          ovr = pf[32 * r:32 * r + 32, 0:SP].rearrange("p (b j) -> p b j", j=J)
                          for ti in range(J):
                              for part, Sx in ((0, Sre), (1, Sim)):
                                  lastmm = (r == 3 and ti == J - 1 and part == 1)
                                  fns.append(lambda g, pair=pair, ti=ti, part=part, Sx=Sx, ovr=ovr, lastmm=lastmm, r=r: g.matmul(
                                      ovr[:, :, ti], Cl[:, pair, ti, part, :], Sx[:, pair, :], start=False, stop=True, skip_group_check=True,
                                      tile_position=(0, 32 * r)))
                      A.group("pe", fns, ["Ktap", "uT", "Cl", "Sre", "Sim"], [pk])
                      STT(y5a[:, q, :], uT[:, q, :], ppc("s5d", q), pf[:, 0:SP], ALU.mult, ALU.add, ["uT", "pp", pk], ["y5a"])
                  stg(2.8)
                  ACT(y5a[:], y5a[:], AF.Gelu_apprx_tanh, ["y5a"], ["y5a"])
                  A.op("act", lambda g: g.copy(y5b[:], y5a[:]), ["y5a"], ["y5b"])
                  for jt in range(4):
                      pf, pk = nextF()
                      A.group("pe", [(lambda g, kt=kt: g.matmul(pf[:, 0:SP], wglu[:, kt, jt * 128:(jt + 1) * 128], y5b[:, kt, :],
                                                                start=(kt == 0), stop=(kt == 3))) for kt in range(4)],
                              ["wglu", "y5b"], [pk])
                      ACT(tA[:, 0:SP], pf[:, 0:SP], AF.Tanh, [pk, "hbglu"], ["tA"], bias=hbglu[:, jt:jt + 1], scale=0.5)
                      TS("dve", tA[:, 0:SP], tA[:, 0:SP], 0.5, ALU.mult, ["tA"], ["tA"], s2=0.5, op1=ALU.add)
                      TT("dve", tA[:, 0:SP], tA[:, 0:SP], y5a[:, jt, :], ALU.mult, ["tA", "y5a"], ["tA"])
                      TT("dve", yTs5[:, jt, :], tA[:, 0:SP], g5[:, jt, :], ALU.mult, ["tA", "g5"], ["yTs5"])
                      if debug and s == 0 and l == 0:
                          TT("dve", tB[:, 0:SP], tA[:, 0:SP], g5[:, jt, :], ALU.mult, ["tA", "g5"], ["tB"])
                          A.dma("sp", "dbg", dbg["ys5"][:, jt, :], tB[:, 0:SP], reads=["tB"], writes=["dbg_out"])

                  stg(3)
                  for c in range(NCH):
                      cs_ = slice(c * T, (c + 1) * T)
                      gc = s * NCH + c
                      dk, dak, vk = "dt%d" % c, "dtA%d" % c, "vtok%d" % c
                      fns = []
                      for tl in range(8):
                          for k in range(4):
                              fns.append(lambda g, tl=tl, k=k: g.matmul(pC[:, tl * 128:(tl + 1) * 128], xbcT[:, tl, c * T + k:c * T + k + T],
                                                                        diagw[:, tl, k, :], start=(k == 0), stop=False))
                          fns.append(lambda g, tl=tl: g.matmul(pC[:, tl * 128:(tl + 1) * 128], onesb[0:1, :], cbrow[0:1, tl * 128:(tl + 1) * 128],
                                                               start=False, stop=True))
                      A.group("pe", fns, ["xbcT", "diagw", "cbf", "cbrow"], ["pC"])
                      ACT(xstok[:], pC[:], AF.Silu, ["pC"], ["xstok"])
                      pf, pk = nextF()
                      fns = []
                      for i2 in range(2):
                          tl = 8 + i2
                          for k in range(4):
                              fns.append(lambda g, tl=tl, k=k, i2=i2: g.matmul(pf[:, i2 * 128:(i2 + 1) * 128], xbcT[:, tl, c * T + k:c * T + k + T],
                                                                               diagw[:, tl, k, :], start=(k == 0), stop=False))
                          fns.append(lambda g, tl=tl, i2=i2: g.matmul(pf[:, i2 * 128:(i2 + 1) * 128], onesb[0:1, :], cbrow[0:1, tl * 128:(tl + 1) * 128],
                                                                      start=False, stop=True))
                      A.group("pe", fns, ["xbcT", "diagw", "cbf", "cbrow"], [pk])
                      ACT(Btok[:], pf[:, 0:256], AF.Silu, [pk], ["Btok"])
                      pf, pk = nextF()
                      A.op("pe", lambda g: g.matmul(pf[:, 0:16], U32, dtA[c][:], start=True, stop=True), ["c32", dak], [pk])
                      ACT(wls[:], pf[:, 0:16], AF.Exp, [pk], ["wls"])
                      TT("dve", wls[:], wls[:], dtt[c][:], ALU.mult, ["wls", dk], ["wls"])
                      TT("dve", xsw[:].rearrange("p (h d) -> p h d", h=16), xstok[:].rearrange("p (h d) -> p h d", h=16),
                         bc(wls[:].unsqueeze(2), [P, 16, 64]), ALU.mult, ["xstok", "wls"], ["xsw"])
                      pf, pk = nextF()
                      A.group("pe", [(lambda g, gg=gg: g.matmul(pf[:, gg * 128:(gg + 1) * 128], BCT[:, gg, cs_], BCT[:, 2 + gg, cs_],
                                                                start=True, stop=True)) for gg in range(2)], ["BCT"], [pk])
                      TT("dve", cbTm[:], pf[:, 0:256].rearrange("p (g t) -> p g t", g=2), bc(tri32.unsqueeze(1), [P, 2, 128]), ALU.mult,
                         [pk, "c32"], ["cbTm"])
                      for qd in range(4):
                          gg = qd // 2
                          pq = qd % 2
                          Dq, dec, eac, Mq, CTs = Dq_2[pq], dec_2[pq], eac_2[pq], Mq_2[pq], CTs_2[pq]
                          kDq, kdecq, keac, kMq, kCTs = "Dq0", "dec%d" % pq, "eac%d" % pq, "Mq%d" % pq, "CTs%d" % pq
                          TT("dve", Dq[:], bc(tri32.unsqueeze(1), [P, 4, 128]), bc(dtA[c][:, 4 * qd:4 * qd + 4].unsqueeze(2), [P, 4, 128]),
                             ALU.mult, ["c32", dak], [kDq])
                          Dq2 = Dq[:].rearrange("p h t -> p (h t)")
                          A.op("pe", lambda g: g.matmul(pS[:], U32, Dq2, start=True, stop=True), ["c32", kDq], ["pS"])
                          ACT(dec[:].rearrange("p h t -> p (h t)"), pS[:], AF.Exp, ["pS"], [kdecq])
                          A.op("pe", lambda g: g.matmul(pTf[:], ones32, Dq2, start=True, stop=True), ["c32", kDq], ["pT"])
                          ACT(eac[:].rearrange("p h t -> p (h t)"), pTf[:], AF.Exp, ["pT"], [keac])
                          for hh in range(4):
                              h = 4 * qd + hh
                              STT(Mq[:, hh, :], dec[:, hh, :], dtt[c][:, h:h + 1], cbTm[:, gg, :], ALU.mult, ALU.mult,
                                  [kdecq, dk, "cbTm"], [kMq])
                          TT("dve", CTs[:], bc(BCT[:, 2 + gg, cs_].unsqueeze(1), [P, 4, 128]), eac[:], ALU.mult, ["BCT", keac], [kCTs])
                          fns = []
                          for hh in range(4):
                              h = 4 * qd + hh
                              po = pY[64 * (h % 2):64 * (h % 2) + 64, (h // 2) * 128:(h // 2 + 1) * 128]
                              tp = (0, 64 * (h % 2))
                              fns.append(lambda g, h=h, hh=hh, po=po, tp=tp: g.matmul(po, xstok[:, h * 64:(h + 1) * 64], Mq[:, hh, :],
                                                                                      start=True, stop=False, tile_position=tp))
                              fns.append(lambda g, h=h, po=po, tp=tp: g.matmul(po, xstok[:, h * 64:(h + 1) * 64], dI[:, h, :],
                                                                               start=False, stop=False, tile_position=tp))
                              fns.append(lambda g, h=h, hh=hh, po=po, tp=tp: g.matmul(po, prevTb[:, h * 64:(h + 1) * 64], CTs[:, hh, :],
                                                                                      start=False, stop=True, tile_position=tp))
                          A.group("pe", fns, ["xstok", kMq, "dI", "prevTb", kCTs], ["pY"])
                          pvq = prevT[:, qd * 256:(qd + 1) * 256].rearrange("p (h d) -> p h d", h=4)
                          TT("dve", pvq, pvq, bc(eac[:, :, 127:128], [P, 4, 64]), ALU.mult, ["prevT", keac], ["prevT"])
                      A.group("pe", [(lambda g, gg=gg: g.matmul(pC[:, gg * 512:(gg + 1) * 512], Btok[:, gg * 128:(gg + 1) * 128],
                                                                xsw[:, gg * 512:(gg + 1) * 512], start=True, stop=True)) for gg in range(2)],
                              ["Btok", "xsw"], ["pC"])
                      TT("dve", prevT[:], prevT[:], pC[:], ALU.add, ["prevT", "pC"], ["prevT"])
                      A.op("act", lambda g: g.copy(prevTb[:], prevT[:]), ["prevT"], ["prevTb"])
                      TT("dve", y1[:], pY[:].rearrange("p (a t) -> p a t", a=8), zs[:, :, cs_], ALU.mult, ["pY", "zs"], ["y1"])
                      ACT(ysq[:], y1[:], AF.Square, ["y1"], ["ysq"])
                      pf, pk = nextF()
                      fns = []
                      for gg in range(2):
                          for i in range(4):
                              fns.append(lambda g, gg=gg, i=i: g.matmul(pf[:, gg * 128:(gg + 1) * 128], onesb, ysq[:, 4 * gg + i, :],
                                                                        start=(i == 0), stop=(i == 3)))
                      A.group("pe", fns, ["cbf", "ysq"], [pk])
                      r2 = rstd2[:].rearrange("p g t -> p (g t)")
                      ACT(r2, pf[:, 0:256], AF.Ln, [pk], ["rstd2"], bias=EPS, scale=1.0 / 512)
                      ACT(r2, r2, AF.Exp, ["rstd2"], ["rstd2"], scale=-0.5)
                      TT("dve", y1[:], y1[:], bc(ppc("ssdnw", 0, 8).unsqueeze(2), [P, 8, 128]), ALU.mult, ["y1", "pp"], ["y1"])
                      for gg in range(2):
                          TT("dve", yTssd[:, 4 * gg:4 * gg + 4, cs_], y1[:, 4 * gg:4 * gg + 4, :], bc(rstd2[:, gg:gg + 1, :], [P, 4, 128]), ALU.mult,
                             ["y1", "rstd2"], ["yTssd"])
                      if debug and s == 0 and l == 0:
                          CP("dve", dbt, yTssd[:, :, cs_], ["yTssd"], ["tB"])
                          A.dma("sp", "dbg", dbg["yssd"][:, :, cs_], dbt, reads=["tB"], writes=["dbg_out"])

                      stg(4)
                      TS("dve", cosT[:], sin_ti, sc_t[:, gc:gc + 1], ALU.mult, ["c32"], ["cosT"])
                      STT(cosT[:], cos_ti, cc_t[:, gc:gc + 1], cosT[:], ALU.mult, ALU.subtract, ["c32", "cosT"], ["cosT"])
                      TS("dve", sinT[:], cos_ti, sc_t[:, gc:gc + 1], ALU.mult, ["c32"], ["sinT"])
                      STT(sinT[:], sin_ti, cc_t[:, gc:gc + 1], sinT[:], ALU.mult, ALU.add, ["c32", "sinT"], ["sinT"])
                      for which, XT_, xkey in (("q", qT, "qT"), ("k", kT, "kT")):
                          for tl in range(2):
                              pf, pk = nextF()
                              A.op("pe", lambda g: g.matmul(pf[:, 0:128], permb, XT_[:, tl, cs_], start=True, stop=True), ["cbf", xkey], [pk])
                              TT("dve", tA[:, 0:128], XT_[:, tl, cs_], cosT[:], ALU.mult, [xkey, "cosT"], ["tA"])
                              TT("dve", tB[:, 0:128], pf[:, 0:128], sinT[:], ALU.mult, [pk, "sinT"], ["tB"])
                              if which == "q":
                                  TT("dve", tA[:, 0:128], tA[:, 0:128], tB[:, 0:128], ALU.add, ["tA", "tB"], ["tA"])
                                  A.op("act", lambda g: g.copy(qb[:, tl, :], tA[:, 0:128]), ["tA"], ["qb"])
                                  TT("dve", qdb[:, tl, :], tA[:, 0:128], qdec[:, tl, :], ALU.mult, ["tA", "c32"], ["qdb"])
                              else:
                                  TT("dve", kb[:, tl, :], tA[:, 0:128], tB[:, 0:128], ALU.add, ["tA", "tB"], ["kb"])
                      A.group("pe", [(lambda g, tl=tl: g.transpose(pT[:, tl * 128:(tl + 1) * 128], kb[:, tl, :], identb)) for tl in range(2)],
                              ["kb", "cbf"], ["pT"])
                      TT("dve", kdtok[:].rearrange("p (h d) -> p h d", h=8), pT[:, 0:256].rearrange("p (h d) -> p h d", h=8),
                         bc(kdec.unsqueeze(2), [P, 8, 32]), ALU.mult, ["pT", "c32"], ["kdtok"])
                      fns = []
                      for h in range(8):
                          r, tl = h % 4, h // 4
                          fns.append(lambda g, h=h, r=r, tl=tl: g.matmul(Gb[r][0][:, tl * 128:(tl + 1) * 128], kb[32 * r:32 * r + 32, tl, :],
                                                                         qb[32 * r:32 * r + 32, tl, :], start=True, stop=True,
                                                                         tile_position=(32 * r, 0)))
                      A.group("pe", fns, ["kb", "qb"], ["pF0", "pF1", "pS", "pC"])
                      for r in range(4):
                          TT("dve", ST[:, r:8:4, :], Gb[r][0][:, 0:256].rearrange("p (a t) -> p a t", a=2), dmat[:, r:8:4, :], ALU.mult,
                             [Gb[r][1], "c32"], ["ST"])
                      pfy, pky = nextF()
                      fns = []
                      for h in range(8):
                          r, tl = h % 4, h // 4
                          po = pfy[64 * (h % 2):64 * (h % 2) + 64, (h // 2) * 128:(h // 2 + 1) * 128]
                          fns.append(lambda g, h=h, po=po: g.matmul(po, vtok[c][:, h * 64:(h + 1) * 64], ST[:, h, :], start=True, stop=False,
                                                                    tile_position=(0, 64 * (h % 2))))
                          fns.append(lambda g, h=h, po=po, r=r, tl=tl: g.matmul(po, rspad[:, h, :], qdb[:, tl, :],
                                                                                start=False, stop=True, tile_position=(0, 64 * (h % 2))))
                      A.group("pe", fns, [vk, "ST", "rspad", "qdb"], [pky])
                      pf, pk = nextF()
                      fns = []
                      for h in range(8):
                          r, tl = h % 4, h // 4
                          fns.append(lambda g, h=h, r=r, tl=tl: g.matmul(pf[32 * r:32 * r + 32, tl * 64:(tl + 1) * 64], kdtok[:, h * 32:(h + 1) * 32],
                                                                         vtok[c][:, h * 64:(h + 1) * 64], start=True, stop=True,
                                                                         tile_position=(0, 32 * r)))
                      A.group("pe", fns, ["kdtok", vk], [pk])
                      for tl in range(2):
                          STT(rstate[:, tl, :], rstate[:, tl, :], gch[:, tl:tl + 1], pf[:, tl * 64:(tl + 1) * 64], ALU.mult, ALU.add,
                              ["rstate", "c32", pk], ["rstate"])
                      for h in range(8):
                          TS("pool", rspad[:, h, :], rstate[:, h // 4, :], rmask[:, h % 4:h % 4 + 1], ALU.mult, ["rstate", "c32"], ["rspad"])
                      CP("dve", yr1[:].rearrange("p a t -> p (a t)"), pfy[:], [pky], ["yr1"])
                      ACT(yrsq[:], yr1[:], AF.Square, ["yr1"], ["yrsq"])
                      pf, pk = nextF()
                      A.group("pe", [(lambda g, i=i: g.matmul(pf[:, i * 128:(i + 1) * 128], blk64b, yrsq[:, i, :], start=True, stop=True))
                                     for i in range(4)], ["cbf", "yrsq"], [pk])
                      r4 = rstd4[:].rearrange("p a t -> p (a t)")
                      ACT(r4, pf[:], AF.Ln, [pk], ["rstd4"], bias=EPS, scale=1.0 / 64)
                      ACT(r4, r4, AF.Exp, ["rstd4"], ["rstd4"], scale=-0.5)
                      TT("dve", yr1[:], yr1[:], bc(ppc("retnw", 0, 4).unsqueeze(2), [P, 4, 128]), ALU.mult, ["yr1", "pp"], ["yr1"])
                      TT("dve", yr1[:], yr1[:], rstd4[:], ALU.mult, ["yr1", "rstd4"], ["yr1"])
                      TT("dve", yTret[:, :, cs_], yr1[:], rg[:, :, cs_], ALU.mult, ["yr1", "rg"], ["yTret"])
                      if debug and s == 0 and l == 0:
                          TT("dve", dbt[:, 0:4, :], yr1[:], rg[:, :, cs_], ALU.mult, ["yr1", "rg"], ["tB"])
                          A.dma("sp", "dbg", dbg["yret"][:, :, cs_], dbt[:, 0:4, :], reads=["tB"], writes=["dbg_out"])

                  stg(5)
                  acc = {(0, 0): (pC[:, 0:512], "pC"), (0, 1): (pC[:, 512:1024], "pC"), (1, 0): (pY[:, 0:512], "pY"), (1, 1): (pY[:, 512:1024], "pY")}
                  for go in range(8):
                      si = (NG + go) % NSLOT
                      wk = "ws%d" % si
                      wv = wslot[si][:].rearrange("p a b -> p (a b)").rearrange("p (a b) -> p a b", a=2)
                      A.dma("sp", wk, wv, woscr_d[go], reads=["woscr%d" % go], writes=[wk])
                      fns = []
                      for c in range(NCH):
                          cs_ = slice(c * T, (c + 1) * T)
                          for half in range(2):
                              for k2 in range(2):
                                  kt = 2 * go + k2
                                  if kt < 8:
                                      lt = yTssd[:, kt, cs_]
                                  elif kt < 12:
                                      lt = yTs5[:, kt - 8, cs_]
                                  else:
                                      lt = yTret[:, kt - 12, cs_]
                                  fns.append(lambda g, c=c, half=half, k2=k2, kt=kt, lt=lt: g.matmul(
                                      acc[(c, half)][0], lt, wv[:, k2, half * 512:(half + 1) * 512], start=(kt == 0), stop=(kt == 15)))
                      A.group("pe", fns, ["yTssd", "yTs5", "yTret", wk], ["pC", "pY"])
                  for c in range(NCH):
                      rows = slice(t0 + c * T, t0 + (c + 1) * T)
                      pacc, pkey = (pC, "pC") if c == 0 else (pY, "pY")
                      A.dma("sp", "hnld", hn[:], src_d[rows, :], writes=["hn"])
                      TT("dve", hn[:], hn[:], pacc[:], ALU.add, ["hn", pkey], ["hn"])
                      if not last:
                          A.dma("sp", "st", hres_d[rows, :], hn[:], reads=["hn"], writes=["hres"])
                      else:
                          ACT(xn[:], hn[:], AF.Square, ["hn"], ["xn", "small"], accum=small[:, 4:5])
                          ACT(small[:, 5:6], small[:, 4:5], AF.Ln, ["small"], ["small"], bias=EPS, scale=1.0 / DM)
                          ACT(small[:, 6:7], small[:, 5:6], AF.Exp, ["small"], ["small"], scale=-0.5)
                          STT(hn[:], hn[:], small[:, 6:7], fnw[:], ALU.mult, ALU.mult, ["hn", "small", "fnw"], ["hn"])
                          A.dma("sp", "st", out_d[rows, :], hn[:], reads=["hn"], writes=["outd"])
                  CP("dve", xbcT[:, :, 0:3], xbcT[:, :, SP:SP + 3], ["xbcT"], ["xbcT"])
              A.barrier()
              es3.close()
              open_stacks[:] = []

        except _Stop:
            for st_ in reversed(open_stacks):
                st_.close()
        A.finish("sp")
        build_program.last_nins = A.nins
    return nc


def make_consts():
    c32 = np.zeros((P, NC32), np.float64)
    k = np.arange(128)
    c32[:, C32["U"]:C32["U"] + 128] = (k[:, None] > k[None, :])
    c32[:, C32["ones"]:C32["ones"] + 128] = 1.0
    c32[:, C32["tri"]:C32["tri"] + 128] = (k[:, None] <= k[None, :])
    log_g = np.log1p(-np.exp2(-5.0 - np.arange(8)))
    scale = 32 ** -0.5
    diff = k[None, :] - k[:, None]
    for h in range(8):
        c32[:, C32["dmat"] + h * 128:C32["dmat"] + (h + 1) * 128] = np.where(diff >= 0, np.exp(np.maximum(diff, 0) * log_g[h]) * scale, 0.0)
    for tl in range(2):
        for h4 in range(4):
            h = 4 * tl + h4
            c32[32 * h4:32 * h4 + 32, C32["qdec"] + tl * 128:C32["qdec"] + (tl + 1) * 128] = np.exp((k + 1.0) * log_g[h])[None, :]
            c32[32 * h4:32 * h4 + 32, C32["gch"] + tl] = np.exp(128.0 * log_g[h])
    for h in range(8):
        c32[:, C32["kdec"] + h] = np.exp((127.0 - k) * log_g[h]) * scale
    inv_freq = 10000.0 ** (-np.arange(0, 32, 2) / 32.0)
    fr = inv_freq[np.arange(128) % 16]
    c32[:, C32["cos"]:C32["cos"] + 128] = np.cos(fr[:, None] * k[None, :])
    c32[:, C32["sin"]:C32["sin"] + 128] = np.sin(fr[:, None] * k[None, :])
    cidx = np.arange(64)
    c32[:, C32["cc"]:C32["cc"] + 64] = np.cos(fr[:, None] * 128.0 * cidx[None, :])
    c32[:, C32["sc"]:C32["sc"] + 64] = np.sin(fr[:, None] * 128.0 * cidx[None, :])
    for r in range(4):
        c32[32 * r:32 * r + 32, C32["rmask"] + r] = 1.0
    cbf = np.zeros((P, 512), np.float32)
    cbf[:, 0:128] = np.eye(128)
    cbf[:, 128:256] = 1.0
    perm = np.zeros((128, 128), np.float32)
    for m in range(128):
        if m % 32 < 16:
            perm[m + 16, m] = -1.0
        else:
            perm[m - 16, m] = 1.0
    cbf[:, 256:384] = perm
    blk = np.zeros((128, 128), np.float32)
    blk[0:64, 0:64] = 1.0
    blk[64:128, 64:128] = 1.0
    cbf[:, 384:512] = blk
    return c32.astype(np.float32), cbf


def prep_weights(inp):
    f = lambda a: np.asarray(a, dtype=np.float32)
    w_in = f(inp["w_in"])
    win_g = np.zeros((2, NG, P, 8, 256), np.float32)
    for l in range(2):
        wl = w_in[l].reshape(8, 128, 5136)
        for gi in range(18):
            for tt in range(2):
                c0 = FM_ORIG[2 * gi + tt]
                win_g[l, gi, :, :, tt * 128:(tt + 1) * 128] = wl[:, :, c0:c0 + 128].transpose(1, 0, 2)
        for hv in range(2):
            win_g[l, 18 + hv] = wl[:, :, 4112 + hv * 256:4112 + (hv + 1) * 256].transpose(1, 0, 2)
        win_g[l, 20, :, :, 0:16] = wl[:, :, 2560:2576].transpose(1, 0, 2)
    wout_h = f(inp["w_out"]).reshape(2, 16, 128, DM).transpose(0, 2, 1, 3).copy()
    wglu_h = f(inp["s5_w_glu"]).reshape(2, 4, 128, 512).transpose(0, 2, 1, 3).copy()
    pp = np.zeros((2, P, NPP), np.float32)
    pb = np.zeros((2, P, NPB), np.float32)
    for l in range(2):
        def put(name, arr):
            arr = np.asarray(arr, np.float32)
            if name in OFFB:
                pb[l, :, OFFB[name]:OFFB[name] + arr.shape[1]] = arr
            else:
                pp[l, :, OFF[name]:OFF[name] + arr.shape[1]] = arr
        put("normw", f(inp["norm_w"])[l].reshape(8, 128).T)
        cw = f(inp["conv_w"])[l]
        put("convw", cw.reshape(4, 12, 128).transpose(2, 1, 0).reshape(128, 48))
        put("convb", f(inp["conv_b"])[l].reshape(12, 128).T)
        put("dtb", np.broadcast_to(f(inp["dt_bias"])[l][None, :], (128, 16)))
        put("alog", np.broadcast_to(f(inp["a_log"])[l][None, :], (128, 16)))
        put("dssd", np.broadcast_to(f(inp["d_ssd"])[l][None, :], (128, 16)))
        put("ssdnw", f(inp["ssd_norm_w"])[l].reshape(8, 128).T)
        put("s5d", f(inp["s5_d"])[l].reshape(4, 128).T)
        put("bglu", f(inp["s5_b_glu"])[l].reshape(4, 128).T)
        put("retnw", f(inp["ret_norm_w"])[l].reshape(4, 128).T)
        lam_re = f(inp["s5_lambda_re"])[l].reshape(16, 2, 64)
        lam_im = f(inp["s5_lambda_im"])[l].reshape(16, 2, 64)
        put("lamre", lam_re.transpose(1, 2, 0).reshape(128, 16))
        put("lamim", lam_im.transpose(1, 2, 0).reshape(128, 16))
        ls = f(inp["s5_log_step"])[l].reshape(16, 2)
        put("lstep", np.broadcast_to(ls.T[:, None, :], (2, 64, 16)).reshape(128, 16))
        for nm, src, tr in (("bre", "s5_b_re", False), ("bim", "s5_b_im", False), ("cre", "s5_c_re", True), ("cim", "s5_c_im", True)):
            a = f(inp[src])[l]
            if tr:
                a = a.transpose(0, 2, 1)
            a = a.reshape(16, 2, 64, 16)
            o = np.zeros((2, 64, 16, 2, 16), np.float32)
            for par in range(2):
                o[par, :, :, par, :] = a[:, par].transpose(1, 0, 2)
            put(nm, o.reshape(128, 512))
    cbrow = f(inp["conv_b"])[:, None, :1280].copy()
    fnw = f(inp["final_norm_w"])[None, :].copy()
    c32, cbf = make_consts()
    return {"win_g": win_g, "wout_h": wout_h, "wglu_h": wglu_h, "pp": pp, "pb": pb, "cbrow": cbrow, "fnw": fnw,
            "cst32": c32, "cstbf": cbf}


_prog_cache = {}


def kernel(**inputs):
    x = np.asarray(inputs["x"], dtype=np.float32)
    B, Lx, _ = x.shape
    nspan = Lx // SP
    shared = prep_weights(inputs)
    key = (nspan, 2)
    if key not in _prog_cache:
        _prog_cache[key] = build_program(nspan=nspan, nlayer=2)
    nc = _prog_cache[key]
    ncores = 8
    in_maps = []
    for cidx in range(ncores):
        m = dict(shared)
        m["x"] = np.ascontiguousarray(x[cidx % B])
        in_maps.append(m)
    res = run_bass_kernel_spmd(nc, in_maps, core_ids=list(range(ncores)))
    out = np.stack([np.asarray(res.results[b]["out"], dtype=np.float32) for b in range(B)], axis=0)
    return out.astype(inputs["x"].dtype)
```

```python
import math
import numpy as np
from contextlib import ExitStack
import concourse.bass as bass
import concourse.mybir as mybir
from concourse.bass_utils import run_bass_kernel_spmd

F32 = mybir.dt.float32
BF16 = mybir.dt.bfloat16
AF = mybir.ActivationFunctionType
ALU = mybir.AluOpType

P = 128
T = 128
NCH = 2
SP = T * NCH
J = 4
NB = SP // J
DM = 1024
EPS = 1e-6
NG = 21
SEQ = 8192
TWO_PI = 2.0 * math.pi

FM_ORIG = ([0 + 128 * i for i in range(8)] + [1024 + 128 * i for i in range(12)] +
           [2576 + 128 * i for i in range(4)] + [3088 + 128 * i for i in range(4)] +
           [3600, 3728] + [3856, 3984] + [4624 + 128 * i for i in range(4)])
assert len(FM_ORIG) == 36

_pp_fields = [("normw", 8), ("convw", 48), ("convb", 12), ("dtb", 16), ("alog", 16), ("dssd", 16),
              ("ssdnw", 8), ("s5d", 4), ("bglu", 4), ("retnw", 4), ("lamre", 16), ("lamim", 16),
              ("lstep", 16)]
_pb_fields = [("bre", 512), ("bim", 512), ("cre", 512), ("cim", 512)]
OFF = {}
_o = 0
for _n, _w in _pp_fields:
    OFF[_n] = _o
    _o += _w
NPP = _o
OFFB = {}
_o = 0
for _n, _w in _pb_fields:
    OFFB[_n] = _o
    _o += _w
NPB = _o

_c32_fields = [("U", 128), ("ones", 128), ("tri", 128), ("dmat", 1024), ("qdec", 256), ("kdec", 8),
               ("gch", 2), ("cos", 128), ("sin", 128), ("cc", 64), ("sc", 64), ("rmask", 4)]
C32 = {}
_o = 0
for _n, _w in _c32_fields:
    C32[_n] = _o
    _o += _w
NC32 = _o


class AS:
    def __init__(self, nc, es):
        self.nc = nc
        self.es = es
        self.eng = {"pe": nc.tensor, "act": nc.scalar, "dve": nc.vector, "pool": nc.gpsimd, "sp": nc.sync}
        self.sem = {k: es.enter_context(nc.semaphore("sem_" + k)) for k in self.eng}
        self.cnt = {k: 0 for k in self.eng}
        self.waited = {k: {} for k in self.eng}
        self.lastw = {}
        self.readers = {}
        self.dsem = {}
        self.nins = 0

    def _need(self, e, deps):
        best = {}
        for (src, val) in deps:
            if self.waited[e].get(src, 0) >= val:
                continue
            if best.get(src, 0) < val:
                best[src] = val
        for src, val in best.items():
            sem = self.dsem[src][0] if src in self.dsem else self.sem[src]
            self.eng[e].wait_ge(sem, val)
            self.waited[e][src] = val
            self.nins += 1

    def _deps(self, reads, writes):
        deps = []
        for k in reads:
            w = self.lastw.get(k)
            if w is not None:
                deps.append(w)
        for k in writes:
            w = self.lastw.get(k)
            if w is not None:
                deps.append(w)
            deps.extend(self.readers.get(k, ()))
        return deps

    def _commit(self, tag, reads, writes):
        for k in reads:
            lst = self.readers.setdefault(k, [])
            lst[:] = [t for t in lst if t[0] != tag[0]]
            lst.append(tag)
        for k in writes:
            self.lastw[k] = tag
            self.readers[k] = []

    def op(self, e, fn, reads=(), writes=()):
        self._need(e, self._deps(reads, writes))
        ins = fn(self.eng[e])
        self.cnt[e] += 1
        ins.then_inc(self.sem[e], 1)
        self.nins += 1
        self._commit((e, self.cnt[e]), reads, writes)

    def group(self, e, fns, reads=(), writes=()):
        self._need(e, self._deps(reads, writes))
        ins = None
        for fn in fns:
            ins = fn(self.eng[e])
            self.nins += 1
        self.cnt[e] += 1
        ins.then_inc(self.sem[e], 1)
        self._commit((e, self.cnt[e]), reads, writes)

    def dma(self, q, chan, out, in_, reads=(), writes=(), **kw):
        if chan not in self.dsem:
            self.dsem[chan] = [self.es.enter_context(self.nc.semaphore("dsem_" + chan)), 0]
        self._need(q, self._deps(reads, writes))
        if q == "pool":
            kw.setdefault("max_dma_last_dim", 2048)
        ins = self.eng[q].dma_start(out=out, in_=in_, **kw)
        self.dsem[chan][1] += 16
        ins.then_inc(self.dsem[chan][0], 16)
        self.nins += 1
        self._commit((chan, self.dsem[chan][1]), reads, writes)

    def barrier(self):
        for e in self.eng:
            deps = [(s_, self.cnt[s_]) for s_ in self.eng if s_ != e and self.cnt[s_] > 0]
            deps += [(ch, v[1]) for ch, v in self.dsem.items() if v[1] > 0]
            self._need(e, deps)

    def finish(self, e="sp"):
        deps = list(self.lastw.values())
        for l in self.readers.values():
            deps.extend(l)
        self._need(e, deps)


import os
VAR_RMAX = int(os.environ.get("K_RMAX", "4"))
VAR_SKIPROT = int(os.environ.get("K_SKIPROT", "0"))


class _Stop(Exception):
    pass


def build_program(nspan=SEQ // SP, nlayer=2, debug=False, stage=None):
    nc = bass.Bass("TRN2", target_bir_lowering=False)

    def stg(k):
        if stage == k:
            raise _Stop()
    L = nspan * SP
    dram = lambda n, s, d, k: nc.dram_tensor(n, s, d, kind=k).ap()
    x_d = dram("x", [L, DM], F32, "ExternalInput")
    win_d = dram("win_g", [2, NG, P, 8, 256], F32, "ExternalInput")
    wout_d = dram("wout_h", [2, P, 16, DM], F32, "ExternalInput")
    wglu_d = dram("wglu_h", [2, P, 4, 512], F32, "ExternalInput")
    pp_d = dram("pp", [2, P, NPP], F32, "ExternalInput")
    pb_d = dram("pb", [2, P, NPB], F32, "ExternalInput")
    cbrow_d = dram("cbrow", [2, 1, 1280], F32, "ExternalInput")
    fnw_d = dram("fnw", [1, DM], F32, "ExternalInput")
    c32_d = dram("cst32", [P, NC32], F32, "ExternalInput")
    cbf_d = dram("cstbf", [P, 512], F32, "ExternalInput")
    out_d = dram("out", [L, DM], F32, "ExternalOutput")
    hres_d = dram("hres", [L, DM], F32, "Internal")
    wscr_d = dram("wscr", [NG, P, 8, 256], BF16, "Internal")
    woscr_d = dram("woscr", [8, P, 2, DM], BF16, "Internal")
    dbg = {}
    if debug:
        dbg["yssd"] = dram("d_yssd", [P, 8, SP], F32, "ExternalOutput")
        dbg["ys5"] = dram("d_ys5", [P, 4, SP], F32, "ExternalOutput")
        dbg["yret"] = dram("d_yret", [P, 4, SP], F32, "ExternalOutput")

    with ExitStack() as es:
        A = AS(nc, es)
        sb = lambda n, s, d: es.enter_context(nc.sbuf_tensor("s_" + n, s, d))
        pst = lambda n, s, d: es.enter_context(nc.psum_tensor("p_" + n, s, d))

        pT = pst("pT", [P, 1024], BF16)
        pF = [pst("pF0", [P, 512], F32), pst("pF1", [P, 512], F32)]
        pS = pst("pS", [P, 512], F32)
        pY = pst("pY", [P, 1024], F32)
        pC = pst("pC", [P, 1024], F32)
        pTf = pT.bitcast(F32)
        pfi = [0]

        def nextF():
            pfi[0] ^= 1
            return pF[pfi[0]], "pF%d" % pfi[0]

        c32 = sb("c32", [P, NC32], F32)
        cbf = sb("cbf", [P, 512], BF16)
        identb = cbf[:, 0:128]
        onesb = cbf[:, 128:256]
        permb = cbf[:, 256:384]
        blk64b = cbf[:, 384:512]
        U32 = c32[:, C32["U"]:C32["U"] + 128]
        ones32 = c32[:, C32["ones"]:C32["ones"] + 128]
        tri32 = c32[:, C32["tri"]:C32["tri"] + 128]
        dmat = c32[:, C32["dmat"]:C32["dmat"] + 1024].rearrange("p (h t) -> p h t", h=8)
        qdec = c32[:, C32["qdec"]:C32["qdec"] + 256].rearrange("p (a t) -> p a t", a=2)
        kdec = c32[:, C32["kdec"]:C32["kdec"] + 8]
        gch = c32[:, C32["gch"]:C32["gch"] + 2]
        cos_ti = c32[:, C32["cos"]:C32["cos"] + 128]
        sin_ti = c32[:, C32["sin"]:C32["sin"] + 128]
        cc_t = c32[:, C32["cc"]:C32["cc"] + 64]
        sc_t = c32[:, C32["sc"]:C32["sc"] + 64]
        rmask = c32[:, C32["rmask"]:C32["rmask"] + 4]
        Gb = [(pF[0], "pF0"), (pF[1], "pF1"), (pS, "pS"), (pC, "pC")]

        pp = sb("pp", [P, NPP], F32)
        ppc = lambda name, i=0, n=1: pp[:, OFF[name] + i:OFF[name] + i + n]
        cbrow = sb("cbrow", [1, 1280], BF16)
        fnw = sb("fnw", [P, DM], F32)
        wglu = sb("wglu", [P, 4, 512], BF16)
        NSLOT = 4
        wslot = [sb("wslot%d" % i, [P, 8, 256], BF16) for i in range(NSLOT)]
        diagw = sb("diagw", [P, 12, 4, 128], BF16)
        dI = sb("dI", [P, 16, 128], BF16)
        Ab = sb("Ab", [P, 16], F32)
        hbglu = sb("hbglu", [P, 4], F32)
        WG = sb("WG", [P, 4, J, 2, 128], BF16)
        Cl = sb("Cl", [P, 16, J, 2, 32], BF16)
        Ktap = sb("Ktap", [P, 4, J, 128], BF16)
        phc = sb("phc", [P, 16, NB + 1], F32)
        phs = sb("phs", [P, 16, NB + 1], F32)
        Rr = sb("Rr", [P, 16], F32)
        prevT = sb("prevT", [P, 1024], F32)
        prevTb = sb("prevTb", [P, 1024], BF16)
        rstate = sb("rstate", [P, 2, 64], F32)
        rstateb = sb("rstateb", [P, 2, 64], BF16)
        rspad = sb("rspad", [P, 8, 64], BF16)
        Vre = sb("Vre", [P, 16, NB + 1], F32)
        Vim = sb("Vim", [P, 16, NB + 1], F32)
        small = sb("small", [P, 16], F32)
        def TT(e, out, a, b, op, r, w):
            A.op(e, lambda g: g.tensor_tensor(out, a, b, op), r, w)

        def TS(e, out, a, s1, op0, r, w, s2=None, op1=None):
            if op1 is None:
                A.op(e, lambda g: g.tensor_scalar(out, a, s1, None, op0), r, w)
            else:
                A.op(e, lambda g: g.tensor_scalar(out, a, s1, s2, op0, op1), r, w)

        def STT(out, a, s, b, op0, op1, r, w):
            A.op("dve", lambda g: g.scalar_tensor_tensor(out, a, s, b, op0, op1), r, w)

        def ACT(out, a, func, r, w, bias=None, scale=None, accum=None):
            kw = {}
            if bias is not None:
                kw["bias"] = bias
            if scale is not None:
                kw["scale"] = scale
            if accum is not None:
                kw["accum_out"] = accum
            A.op("act", lambda g: g.activation(out, a, func, **kw), r, w)

        def CP(e, out, a, r, w):
            A.op(e, lambda g: g.tensor_copy(out, a), r, w)

        def bc(ap, shape):
            return ap.broadcast_to(shape)

        A.dma("sp", "cst", c32[:], c32_d, writes=["c32"])
        A.dma("pool", "cstb", cbf[:], cbf_d, writes=["cbf"])
        A.dma("sp", "cst", fnw[:], fnw_d.partition_broadcast(P), writes=["fnw"])

        open_stacks = []
        try:
          for l in range(nlayer):
              src_d = x_d if l == 0 else hres_d
              last = (l == nlayer - 1)
              A.barrier()
              es2 = ExitStack()
              open_stacks[:] = [es2]
              sb2 = lambda n, s_, d: es2.enter_context(nc.sbuf_tensor("s_%s_L%d" % (n, l), s_, d))
              pb = sb2("pb", [P, NPB], F32)
              pbc = lambda name: pb[:, OFFB[name]:OFFB[name] + 512]
              A.dma("sp", "pb", pb[:], pb_d[l], writes=["s5p"])
              A.dma("sp", "pp", pp[:], pp_d[l], writes=["pp"])
              A.dma("pool", "cbrow", cbrow[:], cbrow_d[l], writes=["cbrow"])
              A.dma("pool", "wglu", wglu[:], wglu_d[l], writes=["wglu"])
              stg(0.1)
              for gi in range(NG):
                  si = gi % NSLOT
                  A.dma("pool", "wsq%d" % si, wslot[si][:], win_d[l, gi], writes=["ws%d" % si])
                  A.dma("sp", "wscr%d" % gi, wscr_d[gi], wslot[si][:], reads=["ws%d" % si], writes=["wscr%d" % gi])
              for go in range(8):
                  si = (NG + go) % NSLOT
                  wv = wslot[si][:].rearrange("p a b -> p (a b)").rearrange("p (a b) -> p a b", a=2)
                  A.dma("pool", "wsq%d" % si, wv, wout_d[l][:, 2 * go:2 * go + 2, :], writes=["ws%d" % si])
                  A.dma("sp", "woscr%d" % go, woscr_d[go], wv, reads=["ws%d" % si], writes=["woscr%d" % go])
              stg(0.2)
              for tl in range(12):
                  for k in range(4):
                      TS("pool", diagw[:, tl, k, :], identb, ppc("convw", tl * 4 + k), ALU.mult, ["cbf", "pp"], ["diagw"])
              for h in range(16):
                  TS("pool", dI[:, h, :], identb, ppc("dssd", h), ALU.mult, ["cbf", "pp"], ["dI"])
              ACT(Ab[:], ppc("alog", 0, 16), AF.Exp, ["pp"], ["Ab"])
              TS("dve", Ab[:], Ab[:], -1.0, ALU.mult, ["Ab"], ["Ab"])
              TS("dve", hbglu[:], ppc("bglu", 0, 4), 0.5, ALU.mult, ["pp"], ["hbglu"])

              stg(0.3)
              s16 = lambda n: sb2("s5_%s" % n, [P, 16], F32)
              step, lrs, ang, mag, sn, cs, angc, msk = [s16(n) for n in ("step", "lrs", "ang", "mag", "sn", "cs", "angc", "msk")]
              lbr, lbi, den, aa, fre, fim, t16a, t16b = [s16(n) for n in ("lbr", "lbi", "den", "aa", "fre", "fim", "t16a", "t16b")]
              K5 = ["s5p"]

              def e16(e, out, a, b, op):
                  TT(e, out, a, b, op, K5, K5)

              ACT(step[:], ppc("lstep", 0, 16), AF.Exp, ["pp"], K5)
              e16("dve", lrs[:], ppc("lamre", 0, 16), step[:], ALU.mult)
              e16("dve", ang[:], ppc("lamim", 0, 16), step[:], ALU.mult)
              ACT(mag[:], lrs[:], AF.Exp, K5, K5)
              ACT(Rr[:], lrs[:], AF.Exp, K5, K5 + ["Rr"], scale=float(J))
              for _ in range(5):
                  TS("dve", msk[:], ang[:], math.pi, ALU.is_gt, K5, K5, s2=TWO_PI, op1=ALU.mult)
                  e16("dve", ang[:], ang[:], msk[:], ALU.subtract)
              TS("dve", angc[:], ang[:], math.pi / 2, ALU.add, K5, K5)
              TS("dve", msk[:], angc[:], math.pi, ALU.is_gt, K5, K5, s2=TWO_PI, op1=ALU.mult)
              e16("dve", angc[:], angc[:], msk[:], ALU.subtract)
              ACT(sn[:], ang[:], AF.Sin, K5, K5)
              ACT(cs[:], angc[:], AF.Sin, K5, K5)
              e16("dve", lbr[:], mag[:], cs[:], ALU.mult)
              e16("dve", lbi[:], mag[:], sn[:], ALU.mult)
              e16("dve", den[:], ppc("lamre", 0, 16), ppc("lamre", 0, 16), ALU.mult)
              e16("dve", t16a[:], ppc("lamim", 0, 16), ppc("lamim", 0, 16), ALU.mult)
              e16("dve", den[:], den[:], t16a[:], ALU.add)
              A.op("dve", lambda g: g.reciprocal(den[:], den[:]), K5, K5)
              TS("dve", aa[:], lbr[:], -1.0, ALU.add, K5, K5)
              e16("dve", t16a[:], aa[:], ppc("lamre", 0, 16), ALU.mult)
              e16("dve", t16b[:], lbi[:], ppc("lamim", 0, 16), ALU.mult)
              e16("dve", fre[:], t16a[:], t16b[:], ALU.add)
              e16("dve", fre[:], fre[:], den[:], ALU.mult)
              e16("dve", t16a[:], lbi[:], ppc("lamre", 0, 16), ALU.mult)
              e16("dve", t16b[:], aa[:], ppc("lamim", 0, 16), ALU.mult)
              e16("dve", fim[:], t16a[:], t16b[:], ALU.subtract)
              e16("dve", fim[:], fim[:], den[:], ALU.mult)
              s512 = lambda n: sb2("s5_%s" % n, [P, 16, 32], F32)
              bbr, bbi, t5a, t5b, ncim = [s512(n) for n in ("bbr", "bbi", "t5a", "t5b", "ncim")]
              b_re = pbc("bre").rearrange("p (a b) -> p a b", a=16)
              b_im = pbc("bim").rearrange("p (a b) -> p a b", a=16)
              c_re = pbc("cre").rearrange("p (a b) -> p a b", a=16)
              c_im = pbc("cim").rearrange("p (a b) -> p a b", a=16)
              b32 = lambda t: bc(t[:].unsqueeze(2), [P, 16, 32])

              def cmul(o_r, o_i, ar, ai, br, bi, sh_b):
                  TT("dve", t5a[:], ar, sh_b(br), ALU.mult, K5, K5)
                  TT("dve", t5b[:], ai, sh_b(bi), ALU.mult, K5, K5)
                  TT("dve", o_r, t5a[:], t5b[:], ALU.subtract, K5, K5)
                  TT("dve", t5a[:], ar, sh_b(bi), ALU.mult, K5, K5)
                  TT("dve", t5b[:], ai, sh_b(br), ALU.mult, K5, K5)
                  TT("dve", o_i, t5a[:], t5b[:], ALU.add, K5, K5)

              cmul(bbr[:], bbi[:], b_re, b_im, fre, fim, b32)
              TS("dve", ncim[:], c_im, -1.0, ALU.mult, K5, K5)
              Xr = [bbr] + [s512("xr%d" % k) for k in range(1, J)]
              Xi = [bbi] + [s512("xi%d" % k) for k in range(1, J)]
              for k in range(1, J):
                  cmul(Xr[k][:], Xi[k][:], Xr[k - 1][:], Xi[k - 1][:], lbr, lbi, b32)
              clr_prev, cli_prev = c_re, c_im
              clr = [s512("clr%d" % k) for k in range(J)]
              cli = [s512("cli%d" % k) for k in range(J)]
              for ti in range(J):
                  cmul(clr[ti][:], cli[ti][:], clr_prev, cli_prev, lbr, lbi, b32)
                  clr_prev, cli_prev = clr[ti][:], cli[ti][:]
                  CP("dve", Cl[:, :, ti, 0, :], clr[ti][:], K5, ["Cl"])
                  TS("dve", Cl[:, :, ti, 1, :], cli[ti][:], -1.0, ALU.mult, K5, ["Cl"])
              Xrb = [sb2("s5_xrb%d" % k, [P, 16, 32], BF16) for k in range(J)]
              Xib = [sb2("s5_xib%d" % k, [P, 16, 32], BF16) for k in range(J)]
              for k in range(J):
                  CP("dve", Xrb[k][:], Xr[k][:], K5, K5)
                  CP("dve", Xib[k][:], Xi[k][:], K5, K5)
              stg(0.4)
              for pair in range(16):
                  q, r = pair // 4, pair % 4
                  for ti in range(J):
                      for part, Xb in ((0, Xrb), (1, Xib)):
                          pf, pk = nextF()
                          A.op("pe", lambda g: g.matmul(pf[32 * r:32 * r + 32, 0:128], Xb[J - 1 - ti][:, pair, :], identb,
                                                         start=True, stop=True, tile_position=(0, 32 * r)),
                               K5 + ["cbf"], [pk])
                          CP("dve", WG[32 * r:32 * r + 32, q, ti, part, :], pf[32 * r:32 * r + 32, 0:128], [pk], ["WG"])
              A.op("pool", lambda g: g.memset(Ktap[:], 0.0), [], ["Ktap"])
              for q in range(4):
                  for j in range(J):
                      pf, pk = nextF()
                      A.group("pe", [
                          lambda g: g.matmul(pf[:, 0:128], Xr[j][:, 4 * q:4 * q + 4, :].rearrange("p a b -> p (a b)"),
                                             c_re[:, 4 * q:4 * q + 4, :].rearrange("p a b -> p (a b)"), start=True, stop=False),
                          lambda g: g.matmul(pf[:, 0:128], Xi[j][:, 4 * q:4 * q + 4, :].rearrange("p a b -> p (a b)"),
                                             ncim[:, 4 * q:4 * q + 4, :].rearrange("p a b -> p (a b)"), start=False, stop=True),
                      ], K5, [pk])
                      for r in range(4):
                          CP("dve", Ktap[32 * r:32 * r + 32, q, j, 32 * r:32 * r + 32], pf[32 * r:32 * r + 32, 32 * r:32 * r + 32], [pk], ["Ktap"])
              eJr, eJi = s16("eJr"), s16("eJi")
              CP("dve", eJr[:], cs[:], K5, K5)
              CP("dve", eJi[:], sn[:], K5, K5)
              for _ in range(J - 1):
                  e16("dve", t16a[:], eJr[:], cs[:], ALU.mult)
                  e16("dve", t16b[:], eJi[:], sn[:], ALU.mult)
                  e16("dve", aa[:], t16a[:], t16b[:], ALU.subtract)
                  e16("dve", t16a[:], eJr[:], sn[:], ALU.mult)
                  e16("dve", t16b[:], eJi[:], cs[:], ALU.mult)
                  e16("dve", eJi[:], t16a[:], t16b[:], ALU.add)
                  CP("dve", eJr[:], aa[:], K5, K5)
              KP = ["ph"]
              A.op("dve", lambda g: g.memset(phc[:, :, 0:1], 1.0), [], KP)
              A.op("dve", lambda g: g.memset(phs[:, :, 0:1], 0.0), [], KP)
              CP("dve", phc[:, :, 1], eJr[:], K5, KP)
              CP("dve", phs[:, :, 1], eJi[:], K5, KP)
              tph = sb2("s5_tph", [P, 16, 32], F32)
              tph2 = sb2("s5_tph2", [P, 16, 32], F32)
              n = 1
              while n < NB:
                  cn = bc(phc[:, :, n:n + 1], [P, 16, n])
                  sn_ = bc(phs[:, :, n:n + 1], [P, 16, n])
                  TT("dve", tph[:, :, 0:n], phc[:, :, 1:n + 1], cn, ALU.mult, KP, KP)
                  TT("dve", tph2[:, :, 0:n], phs[:, :, 1:n + 1], sn_, ALU.mult, KP, KP)
                  TT("dve", phc[:, :, n + 1:2 * n + 1], tph[:, :, 0:n], tph2[:, :, 0:n], ALU.subtract, KP, KP)
                  TT("dve", tph[:, :, 0:n], phc[:, :, 1:n + 1], sn_, ALU.mult, KP, KP)
                  TT("dve", tph2[:, :, 0:n], phs[:, :, 1:n + 1], cn, ALU.mult, KP, KP)
                  TT("dve", phs[:, :, n + 1:2 * n + 1], tph[:, :, 0:n], tph2[:, :, 0:n], ALU.add, KP, KP)
                  n *= 2

              stg(1)
              A.barrier()
              es2.close()
              es3 = ExitStack()
              open_stacks[:] = [es3]
              sb3 = lambda n, s_, d: es3.enter_context(nc.sbuf_tensor("s_%s_L%d" % (n, l), s_, d))
              xt = [sb3("xt0", [P, DM], F32)] * 2
              xn = sb3("xn", [P, DM], BF16)
              hTs = [sb3("hT0", [P, 8, SP], BF16), sb3("hT1", [P, 8, SP], BF16)]
              zs = sb3("zs", [P, 8, SP], BF16)
              xbcT = sb3("xbcT", [P, 12, 3 + SP], BF16)
              g5 = sb3("g5", [P, 4, SP], BF16)
              uT = sb3("uT", [P, 4, SP], BF16)
              qT = sb3("qT", [P, 2, SP], BF16)
              kT = sb3("kT", [P, 2, SP], BF16)
              rg = sb3("rg", [P, 4, SP], BF16)
              vtok = [sb3("vtok%d" % i, [P, 512], BF16) for i in range(NCH)]
              dtt = [sb3("dt%d" % i, [P, 16], F32) for i in range(NCH)]
              dtA = [sb3("dtA%d" % i, [P, 16], F32) for i in range(NCH)]
              BCT = sb3("BCT", [P, 4, SP], BF16)
              yTs5 = sb3("yTs5", [P, 4, SP], BF16)
              cre = sb3("cre", [P, 16, NB], F32)
              cim = sb3("cim", [P, 16, NB], F32)
              Sre = sb3("Sre", [P, 16, NB], BF16)
              Sim = sb3("Sim", [P, 16, NB], BF16)
              tA = sb3("tA", [P, 1024], F32)
              tB = sb3("tB", [P, 1024], F32)
              y5a = sb3("y5a", [P, 4, SP], F32)
              y5b = sb3("y5b", [P, 4, SP], BF16)
              xstok = sb3("xstok", [P, 1024], BF16)
              xsw = sb3("xsw", [P, 1024], BF16)
              Btok = sb3("Btok", [P, 256], BF16)
              cbTm = sb3("cbTm", [P, 2, 128], F32)
              Dq_2 = [sb3("Dq0", [P, 4, 128], F32)] * 2
              dec_2 = [sb3("dec0", [P, 4, 128], F32), sb3("dec1", [P, 4, 128], F32)]
              eac_2 = [sb3("eac0", [P, 4, 128], F32), sb3("eac1", [P, 4, 128], F32)]
              Mq_2 = [sb3("Mq0", [P, 4, 128], BF16), sb3("Mq1", [P, 4, 128], BF16)]
              CTs_2 = [sb3("CTs0", [P, 4, 128], BF16), sb3("CTs1", [P, 4, 128], BF16)]
              y1 = sb3("y1", [P, 8, 128], F32)
              ysq = sb3("ysq", [P, 8, 128], BF16)
              rstd2 = sb3("rstd2", [P, 2, 128], F32)
              yTssd = sb3("yTssd", [P, 8, SP], BF16)
              wls = sb3("wls", [P, 16], F32)
              cosT = sb3("cosT", [P, 128], F32)
              sinT = sb3("sinT", [P, 128], F32)
              qb = sb3("qb", [P, 2, 128], BF16)
              qdb = sb3("qdb", [P, 2, 128], BF16)
              kb = sb3("kb", [P, 2, 128], BF16)
              kdtok = sb3("kdtok", [P, 256], BF16)
              ST = sb3("ST", [P, 8, 128], BF16)
              yr1 = sb3("yr1", [P, 4, 128], F32)
              yrsq = sb3("yrsq", [P, 4, 128], BF16)
              rstd4 = sb3("rstd4", [P, 4, 128], F32)
              yTret = sb3("yTret", [P, 4, SP], BF16)
              hn = sb3("hn", [P, DM], F32)
              dbt = tB[:].rearrange("p (a t) -> p a t", a=8)
              A.op("pool", lambda g: g.memset(prevT[:], 0.0), [], ["prevT"])
              A.op("pool", lambda g: g.memset(prevTb[:], 0.0), [], ["prevTb"])
              A.op("pool", lambda g: g.memset(rstate[:], 0.0), [], ["rstate"])
              A.op("pool", lambda g: g.memset(rstateb[:], 0.0), [], ["rstateb"])
              A.op("pool", lambda g: g.memset(rspad[:], 0.0), [], ["rspad"])
              A.op("pool", lambda g: g.memset(Vre[:], 0.0), [], ["Vre"])
              A.op("pool", lambda g: g.memset(Vim[:], 0.0), [], ["Vim"])
              A.op("pool", lambda g: g.memset(xbcT[:, :, 0:3], 0.0), [], ["xbcT"])

              def front_end(s):
                  t0 = s * SP
                  hT = hTs[s % 2]
                  hk = "hT%d" % (s % 2)
                  for c in range(NCH):
                      xk = "xt0"
                      A.dma("sp", xk, xt[c][:], src_d[t0 + c * T:t0 + (c + 1) * T, :], writes=[xk])
                      ACT(xn[:], xt[c][:], AF.Square, [xk], ["xn", "small"], accum=small[:, 0:1])
                      ACT(small[:, 1:2], small[:, 0:1], AF.Ln, ["small"], ["small"], bias=EPS, scale=1.0 / DM)
                      ACT(small[:, 2:3], small[:, 1:2], AF.Exp, ["small"], ["small"], scale=-0.5)
                      ACT(xn[:], xt[c][:], AF.Copy, [xk, "small"], ["xn"], scale=small[:, 2:3])
                      A.group("pe", [(lambda g, kt=kt: g.transpose(pT[:, kt * 128:(kt + 1) * 128], xn[:, kt * 128:(kt + 1) * 128], identb))
                                     for kt in range(8)], ["xn", "cbf"], ["pT"])
                      TT("dve", hT[:, :, c * T:(c + 1) * T], pT[:].rearrange("p (k t) -> p k t", k=8),
                         bc(ppc("normw", 0, 8).unsqueeze(2), [P, 8, T]), ALU.mult, ["pT", "pp"], [hk])

              front_end(0)
              for s in range(nspan):
                  t0 = s * SP
                  hT = hTs[s % 2]
                  hk = "hT%d" % (s % 2)
                  Gs5 = [(pS, "pS"), (pC[:, 0:512], "pC"), (pC[:, 512:1024], "pC"), (pY[:, 0:512], "pY")]
                  def s5_pre():
                      fns = []
                      for bt in range(4):
                          for part in range(2):
                              for ti in range(J):
                                  for r in range(4):
                                      uv = uT[32 * r:32 * r + 32, bt, :].rearrange("p (b j) -> p b j", j=J)
                                      fns.append(lambda g, r=r, part=part, ti=ti, uv=uv, bt=bt: g.matmul(
                                          Gs5[r][0][:, (bt * 2 + part) * NB:(bt * 2 + part + 1) * NB], WG[32 * r:32 * r + 32, bt, ti, part, :],
                                          uv[:, :, ti], start=(ti == 0), stop=(ti == J - 1), tile_position=(32 * r, 0)))
                      A.group("pe", fns, ["WG", "uT"], ["pS", "pC", "pY"])
                      for r in range(4):
                          gk = Gs5[r][1]
                          Gv = Gs5[r][0][:, 0:512].rearrange("p (t a b) -> p t a b", t=4, a=2)
                          tAv = tA[:, 0:4 * NB].rearrange("p (r b) -> p r b", r=4)
                          tBv = tB[:, 0:4 * NB].rearrange("p (r b) -> p r b", r=4)
                          TT("dve", tAv, Gv[:, :, 0, :], phc[:, r:16:4, 1:NB + 1], ALU.mult, [gk, "ph"], ["tA"])
                          TT("dve", tBv, Gv[:, :, 1, :], phs[:, r:16:4, 1:NB + 1], ALU.mult, [gk, "ph"], ["tB"])
                          TT("dve", cre[:, r:16:4, :], tAv, tBv, ALU.add, ["tA", "tB"], ["cre"])
                          TT("dve", tAv, Gv[:, :, 1, :], phc[:, r:16:4, 1:NB + 1], ALU.mult, [gk, "ph"], ["tA"])
                          TT("dve", tBv, Gv[:, :, 0, :], phs[:, r:16:4, 1:NB + 1], ALU.mult, [gk, "ph"], ["tB"])
                          TT("dve", cim[:, r:16:4, :], tAv, tBv, ALU.subtract, ["tA", "tB"], ["cim"])

                  def s5_scan_all():
                      for pair in range(16):
                          A.op("dve", lambda g: g.tensor_tensor_scan(Vre[:, pair, 1:NB + 1], bc(Rr[:, pair:pair + 1], [P, NB]), cre[:, pair, :],
                                                                     Vre[:, pair, 0:1], ALU.mult, ALU.add), ["Rr", "cre", "Vre"], ["Vre"])
                          A.op("dve", lambda g: g.tensor_tensor_scan(Vim[:, pair, 1:NB + 1], bc(Rr[:, pair:pair + 1], [P, NB]), cim[:, pair, :],
                                                                     Vim[:, pair, 0:1], ALU.mult, ALU.add), ["Rr", "cim", "Vim"], ["Vim"])

                  for gi in range(NG):
                      si = gi % NSLOT
                      wk = "ws%d" % si
                      A.dma("sp", wk, wslot[si][:], wscr_d[gi], reads=["wscr%d" % gi], writes=[wk])
                      if gi < 18:
                          for tt in range(2):
                              ft = 2 * gi + tt
                              pf, pk = nextF()
                              A.group("pe", [(lambda g, kt=kt: g.matmul(pf[:, 0:SP], wslot[si][:, kt, tt * 128:(tt + 1) * 128], hT[:, kt, :],
                                                                        start=(kt == 0), stop=(kt == 7))) for kt in range(8)],
                                      [wk, hk], [pk])
                              if ft < 8:
                                  ACT(zs[:, ft, :], pf[:, 0:SP], AF.Silu, [pk], ["zs"])
                              elif ft < 20:
                                  CP("dve", xbcT[:, ft - 8, 3:3 + SP], pf[:, 0:SP], [pk], ["xbcT"])
                              elif ft < 24:
                                  ACT(g5[:, ft - 20, :], pf[:, 0:SP], AF.Silu, [pk], ["g5"])
                              elif ft < 28:
                                  CP("dve", uT[:, ft - 24, :], pf[:, 0:SP], [pk], ["uT"])
                              elif ft < 30:
                                  A.op("act", lambda g: g.copy(qT[:, ft - 28, :], pf[:, 0:SP]), [pk], ["qT"])
                              elif ft < 32:
                                  A.op("act", lambda g: g.copy(kT[:, ft - 30, :], pf[:, 0:SP]), [pk], ["kT"])
                              else:
                                  ACT(rg[:, ft - 32, :], pf[:, 0:SP], AF.Silu, [pk], ["rg"])
                      if gi == 13:
                          s5_pre()
                      if gi == 17:
                          s5_scan_all()
                      if gi < 18:
                          pass
                      elif gi < 20:
                          hv = gi - 18
                          for c in range(NCH):
                              pf, pk = nextF()
                              A.group("pe", [(lambda g, kt=kt: g.matmul(pf[:, 0:256], hT[:, kt, c * T:(c + 1) * T], wslot[si][:, kt, :],
                                                                        start=(kt == 0), stop=(kt == 7))) for kt in range(8)],
                                      [wk, hk], [pk])
                              A.op("act", lambda g: g.copy(vtok[c][:, hv * 256:(hv + 1) * 256], pf[:, 0:256]), [pk], ["vtok%d" % c])
                      else:
                          for c in range(NCH):
                              pf, pk = nextF()
                              A.group("pe", [(lambda g, kt=kt: g.matmul(pf[:, 0:16], hT[:, kt, c * T:(c + 1) * T], wslot[si][:, kt, 0:16],
                                                                        start=(kt == 0), stop=(kt == 7))) for kt in range(8)],
                                      [wk, hk], [pk])
                              dk = "dt%d" % c
                              TT("dve", dtt[c][:], pf[:, 0:16], ppc("dtb", 0, 16), ALU.add, [pk, "pp"], [dk])
                              ACT(dtt[c][:], dtt[c][:], AF.Exp, [dk], [dk])
                              ACT(dtt[c][:], dtt[c][:], AF.Ln, [dk], [dk], bias=1.0)
                              TT("dve", dtA[c][:], dtt[c][:], Ab[:], ALU.mult, [dk, "Ab"], ["dtA%d" % c])

                  if s + 1 < nspan:
                      front_end(s + 1)
                  stg(2)
                  for i4 in range(4):
                      tl = 8 + i4
                      pf, pk = nextF()
                      A.group("pe", [(lambda g, k=k: g.matmul(pf[:, 0:SP], diagw[:, tl, k, :], xbcT[:, tl, k:k + SP],
                                                              start=(k == 0), stop=(k == 3))) for k in range(4)],
                              ["diagw", "xbcT"], [pk])
                      ACT(BCT[:, i4, :], pf[:, 0:SP], AF.Silu, [pk, "pp"], ["BCT"], bias=ppc("convb", tl))

                  stg(2.5)
                  stg(2.6)
                  tA3 = tA[:].rearrange("p (a b) -> p a b", a=16)
                  tB3 = tB[:].rearrange("p (a b) -> p a b", a=16)
                  TT("dve", tA3, Vre[:, :, 0:NB], phc[:, :, 0:NB], ALU.mult, ["Vre", "ph"], ["tA"])
                  TT("dve", tB3, Vim[:, :, 0:NB], phs[:, :, 0:NB], ALU.mult, ["Vim", "ph"], ["tB"])
                  TT("dve", Sre[:], tA3, tB3, ALU.subtract, ["tA", "tB"], ["Sre"])
                  TT("dve", tA3, Vre[:, :, 0:NB], phs[:, :, 0:NB], ALU.mult, ["Vre", "ph"], ["tA"])
                  TT("dve", tB3, Vim[:, :, 0:NB], phc[:, :, 0:NB], ALU.mult, ["Vim", "ph"], ["tB"])
                  TT("dve", Sim[:], tA3, tB3, ALU.add, ["tA", "tB"], ["Sim"])
                  tcr = tA[:, 0:16]
                  tci = tB[:, 0:16]
                  tc2 = tA[:, 16:32]
                  tc3 = tB[:, 16:32]
                  TT("dve", tcr, Vre[:, :, NB], phc[:, :, NB], ALU.mult, ["Vre", "ph", "Sre", "Sim"], ["tA"])
                  TT("dve", tci, Vim[:, :, NB], phs[:, :, NB], ALU.mult, ["Vim", "ph", "Sre", "Sim"], ["tB"])
                  TT("dve", tc2, Vre[:, :, NB], phs[:, :, NB], ALU.mult, ["Vre", "ph"], ["tA"])
                  TT("dve", tc3, Vim[:, :, NB], phc[:, :, NB], ALU.mult, ["Vim", "ph"], ["tB"])
                  TT("dve", Vre[:, :, 0], tcr, tci, ALU.subtract, ["tA", "tB", "Sre", "Sim"], ["Vre"])
                  TT("dve", Vim[:, :, 0], tc2, tc3, ALU.add, ["tA", "tB", "Sre", "Sim"], ["Vim"])
                  stg(2.7)
                  for q in range(4):
                      pf, pk = nextF()
                      ov = pf[:, 0:SP].rearrange("p (b j) -> p b j", j=J)
                      uvq = uT[:, q, :].rearrange("p (b j) -> p b j", j=J)
                      fns = []
                      fns.append(lambda g: g.matmul(pf[:, 0:SP], Ktap[:, q, 0, :], uT[:, q, :], start=True, stop=True))
                      for j in range(1, J):
                          for ti in range(j, J):
                              fns.append(lambda g, j=j, ti=ti: g.matmul(ov[:, :, ti], Ktap[:, q, j, :], uvq[:, :, ti - j],
                                                                        start=False, stop=True, skip_group_check=True))
                      for r in range(4):
                          pair = 4 * q + r
                          ovr = pf[32 * r:32 * r + 32, 0:SP].rearrange("p (b j) -> p b j", j=J)
                          for ti in range(J):
                              for part, Sx in ((0, Sre), (1, Sim)):
                                  lastmm = (r == 3 and ti == J - 1 and part == 1)
                                  fns.append(lambda g, pair=pair, ti=ti, part=part, Sx=Sx, ovr=ovr, lastmm=lastmm, r=r: g.matmul(
                                      ovr[:, :, ti], Cl[:, pair, ti, part, :], Sx[:, pair, :], start=False, stop=True, skip_group_check=True,
                                      tile_position=(0, 32 * r)))
                      A.group("pe", fns, ["Ktap", "uT", "Cl", "Sre", "Sim"], [pk])
                      STT(y5a[:, q, :], uT[:, q, :], ppc("s5d", q), pf[:, 0:SP], ALU.mult, ALU.add, ["uT", "pp", pk], ["y5a"])
                  stg(2.8)
                  ACT(y5a[:], y5a[:], AF.Gelu_apprx_tanh, ["y5a"], ["y5a"])
                  A.op("act", lambda g: g.copy(y5b[:], y5a[:]), ["y5a"], ["y5b"])
                  for jt in range(4):
                      pf, pk = nextF()
                      A.group("pe", [(lambda g, kt=kt: g.matmul(pf[:, 0:SP], wglu[:, kt, jt * 128:(jt + 1) * 128], y5b[:, kt, :],
                                                                start=(kt == 0), stop=(kt == 3))) for kt in range(4)],
                              ["wglu", "y5b"], [pk])
                      ACT(tA[:, 0:SP], pf[:, 0:SP], AF.Tanh, [pk, "hbglu"], ["tA"], bias=hbglu[:, jt:jt + 1], scale=0.5)
                      TS("dve", tA[:, 0:SP], tA[:, 0:SP], 0.5, ALU.mult, ["tA"], ["tA"], s2=0.5, op1=ALU.add)
                      TT("dve", tA[:, 0:SP], tA[:, 0:SP], y5a[:, jt, :], ALU.mult, ["tA", "y5a"], ["tA"])
                      TT("dve", yTs5[:, jt, :], tA[:, 0:SP], g5[:, jt, :], ALU.mult, ["tA", "g5"], ["yTs5"])
                      if debug and s == 0 and l == 0:
                          TT("dve", tB[:, 0:SP], tA[:, 0:SP], g5[:, jt, :], ALU.mult, ["tA", "g5"], ["tB"])
                          A.dma("sp", "dbg", dbg["ys5"][:, jt, :], tB[:, 0:SP], reads=["tB"], writes=["dbg_out"])

                  stg(3)
                  for c in range(NCH):
                      cs_ = slice(c * T, (c + 1) * T)
                      gc = s * NCH + c
                      dk, dak, vk = "dt%d" % c, "dtA%d" % c, "vtok%d" % c
                      fns = []
                      for tl in range(8):
                          for k in range(4):
                              fns.append(lambda g, tl=tl, k=k: g.matmul(pC[:, tl * 128:(tl + 1) * 128], xbcT[:, tl, c * T + k:c * T + k + T],
                                                                        diagw[:, tl, k, :], start=(k == 0), stop=False))
                          fns.append(lambda g, tl=tl: g.matmul(pC[:, tl * 128:(tl + 1) * 128], onesb[0:1, :], cbrow[0:1, tl * 128:(tl + 1) * 128],
                                                               start=False, stop=True))
                      A.group("pe", fns, ["xbcT", "diagw", "cbf", "cbrow"], ["pC"])
                      ACT(xstok[:], pC[:], AF.Silu, ["pC"], ["xstok"])
                      pf, pk = nextF()
                      fns = []
                      for i2 in range(2):
                          tl = 8 + i2
                          for k in range(4):
                              fns.append(lambda g, tl=tl, k=k, i2=i2: g.matmul(pf[:, i2 * 128:(i2 + 1) * 128], xbcT[:, tl, c * T + k:c * T + k + T],
                                                                               diagw[:, tl, k, :], start=(k == 0), stop=False))
                          fns.append(lambda g, tl=tl, i2=i2: g.matmul(pf[:, i2 * 128:(i2 + 1) * 128], onesb[0:1, :], cbrow[0:1, tl * 128:(tl + 1) * 128],
                                                                      start=False, stop=True))
                      A.group("pe", fns, ["xbcT", "diagw", "cbf", "cbrow"], [pk])
                      ACT(Btok[:], pf[:, 0:256], AF.Silu, [pk], ["Btok"])
                      pf, pk = nextF()
                      A.op("pe", lambda g: g.matmul(pf[:, 0:16], U32, dtA[c][:], start=True, stop=True), ["c32", dak], [pk])
                      ACT(wls[:], pf[:, 0:16], AF.Exp, [pk], ["wls"])
                      TT("dve", wls[:], wls[:], dtt[c][:], ALU.mult, ["wls", dk], ["wls"])
                      TT("dve", xsw[:].rearrange("p (h d) -> p h d", h=16), xstok[:].rearrange("p (h d) -> p h d", h=16),
                         bc(wls[:].unsqueeze(2), [P, 16, 64]), ALU.mult, ["xstok", "wls"], ["xsw"])
                      pf, pk = nextF()
                      A.group("pe", [(lambda g, gg=gg: g.matmul(pf[:, gg * 128:(gg + 1) * 128], BCT[:, gg, cs_], BCT[:, 2 + gg, cs_],
                                                                start=True, stop=True)) for gg in range(2)], ["BCT"], [pk])
                      TT("dve", cbTm[:], pf[:, 0:256].rearrange("p (g t) -> p g t", g=2), bc(tri32.unsqueeze(1), [P, 2, 128]), ALU.mult,
                         [pk, "c32"], ["cbTm"])
                      for qd in range(4):
                          gg = qd // 2
                          pq = qd % 2
                          Dq, dec, eac, Mq, CTs = Dq_2[pq], dec_2[pq], eac_2[pq], Mq_2[pq], CTs_2[pq]
                          kDq, kdecq, keac, kMq, kCTs = "Dq0", "dec%d" % pq, "eac%d" % pq, "Mq%d" % pq, "CTs%d" % pq
                          TT("dve", Dq[:], bc(tri32.unsqueeze(1), [P, 4, 128]), bc(dtA[c][:, 4 * qd:4 * qd + 4].unsqueeze(2), [P, 4, 128]),
                             ALU.mult, ["c32", dak], [kDq])
                          Dq2 = Dq[:].rearrange("p h t -> p (h t)")
                          A.op("pe", lambda g: g.matmul(pS[:], U32, Dq2, start=True, stop=True), ["c32", kDq], ["pS"])
                          ACT(dec[:].rearrange("p h t -> p (h t)"), pS[:], AF.Exp, ["pS"], [kdecq])
                          A.op("pe", lambda g: g.matmul(pTf[:], ones32, Dq2, start=True, stop=True), ["c32", kDq], ["pT"])
                          ACT(eac[:].rearrange("p h t -> p (h t)"), pTf[:], AF.Exp, ["pT"], [keac])
                          for hh in range(4):
                              h = 4 * qd + hh
                              STT(Mq[:, hh, :], dec[:, hh, :], dtt[c][:, h:h + 1], cbTm[:, gg, :], ALU.mult, ALU.mult,
                                  [kdecq, dk, "cbTm"], [kMq])
                          TT("dve", CTs[:], bc(BCT[:, 2 + gg, cs_].unsqueeze(1), [P, 4, 128]), eac[:], ALU.mult, ["BCT", keac], [kCTs])
                          fns = []
                          for hh in range(4):
                              h = 4 * qd + hh
                              po = pY[64 * (h % 2):64 * (h % 2) + 64, (h // 2) * 128:(h // 2 + 1) * 128]
                              tp = (0, 64 * (h % 2))
                              fns.append(lambda g, h=h, hh=hh, po=po, tp=tp: g.matmul(po, xstok[:, h * 64:(h + 1) * 64], Mq[:, hh, :],
                                                                                      start=True, stop=False, tile_position=tp))
                              fns.append(lambda g, h=h, po=po, tp=tp: g.matmul(po, xstok[:, h * 64:(h + 1) * 64], dI[:, h, :],
                                                                               start=False, stop=False, tile_position=tp))
                              fns.append(lambda g, h=h, hh=hh, po=po, tp=tp: g.matmul(po, prevTb[:, h * 64:(h + 1) * 64], CTs[:, hh, :],
                                                                                      start=False, stop=True, tile_position=tp))
                          A.group("pe", fns, ["xstok", kMq, "dI", "prevTb", kCTs], ["pY"])
                          pvq = prevT[:, qd * 256:(qd + 1) * 256].rearrange("p (h d) -> p h d", h=4)
                          TT("dve", pvq, pvq, bc(eac[:, :, 127:128], [P, 4, 64]), ALU.mult, ["prevT", keac], ["prevT"])
                      A.group("pe", [(lambda g, gg=gg: g.matmul(pC[:, gg * 512:(gg + 1) * 512], Btok[:, gg * 128:(gg + 1) * 128],
                                                                xsw[:, gg * 512:(gg + 1) * 512], start=True, stop=True)) for gg in range(2)],
                              ["Btok", "xsw"], ["pC"])
                      TT("dve", prevT[:], prevT[:], pC[:], ALU.add, ["prevT", "pC"], ["prevT"])
                      A.op("act", lambda g: g.copy(prevTb[:], prevT[:]), ["prevT"], ["prevTb"])
                      TT("dve", y1[:], pY[:].rearrange("p (a t) -> p a t", a=8), zs[:, :, cs_], ALU.mult, ["pY", "zs"], ["y1"])
                      ACT(ysq[:], y1[:], AF.Square, ["y1"], ["ysq"])
                      pf, pk = nextF()
                      fns = []
                      for gg in range(2):
                          for i in range(4):
                              fns.append(lambda g, gg=gg, i=i: g.matmul(pf[:, gg * 128:(gg + 1) * 128], onesb, ysq[:, 4 * gg + i, :],
                                                                        start=(i == 0), stop=(i == 3)))
                      A.group("pe", fns, ["cbf", "ysq"], [pk])
                      r2 = rstd2[:].rearrange("p g t -> p (g t)")
                      ACT(r2, pf[:, 0:256], AF.Ln, [pk], ["rstd2"], bias=EPS, scale=1.0 / 512)
                      ACT(r2, r2, AF.Exp, ["rstd2"], ["rstd2"], scale=-0.5)
                      TT("dve", y1[:], y1[:], bc(ppc("ssdnw", 0, 8).unsqueeze(2), [P, 8, 128]), ALU.mult, ["y1", "pp"], ["y1"])
                      for gg in range(2):
                          TT("dve", yTssd[:, 4 * gg:4 * gg + 4, cs_], y1[:, 4 * gg:4 * gg + 4, :], bc(rstd2[:, gg:gg + 1, :], [P, 4, 128]), ALU.mult,
                             ["y1", "rstd2"], ["yTssd"])
                      if debug and s == 0 and l == 0:
                          CP("dve", dbt, yTssd[:, :, cs_], ["yTssd"], ["tB"])
                          A.dma("sp", "dbg", dbg["yssd"][:, :, cs_], dbt, reads=["tB"], writes=["dbg_out"])

                      stg(4)
                      TS("dve", cosT[:], sin_ti, sc_t[:, gc:gc + 1], ALU.mult, ["c32"], ["cosT"])
                      STT(cosT[:], cos_ti, cc_t[:, gc:gc + 1], cosT[:], ALU.mult, ALU.subtract, ["c32", "cosT"], ["cosT"])
                      TS("dve", sinT[:], cos_ti, sc_t[:, gc:gc + 1], ALU.mult, ["c32"], ["sinT"])
                      STT(sinT[:], sin_ti, cc_t[:, gc:gc + 1], sinT[:], ALU.mult, ALU.add, ["c32", "sinT"], ["sinT"])
                      for which, XT_, xkey in (("q", qT, "qT"), ("k", kT, "kT")):
                          for tl in range(2):
                              pf, pk = nextF()
                              A.op("pe", lambda g: g.matmul(pf[:, 0:128], permb, XT_[:, tl, cs_], start=True, stop=True), ["cbf", xkey], [pk])
                              TT("dve", tA[:, 0:128], XT_[:, tl, cs_], cosT[:], ALU.mult, [xkey, "cosT"], ["tA"])
                              TT("dve", tB[:, 0:128], pf[:, 0:128], sinT[:], ALU.mult, [pk, "sinT"], ["tB"])
                              if which == "q":
                                  TT("dve", tA[:, 0:128], tA[:, 0:128], tB[:, 0:128], ALU.add, ["tA", "tB"], ["tA"])
                                  A.op("act", lambda g: g.copy(qb[:, tl, :], tA[:, 0:128]), ["tA"], ["qb"])
                                  TT("dve", qdb[:, tl, :], tA[:, 0:128], qdec[:, tl, :], ALU.mult, ["tA", "c32"], ["qdb"])
                              else:
                                  TT("dve", kb[:, tl, :], tA[:, 0:128], tB[:, 0:128], ALU.add, ["tA", "tB"], ["kb"])
                      A.group("pe", [(lambda g, tl=tl: g.transpose(pT[:, tl * 128:(tl + 1) * 128], kb[:, tl, :], identb)) for tl in range(2)],
                              ["kb", "cbf"], ["pT"])
                      TT("dve", kdtok[:].rearrange("p (h d) -> p h d", h=8), pT[:, 0:256].rearrange("p (h d) -> p h d", h=8),
                         bc(kdec.unsqueeze(2), [P, 8, 32]), ALU.mult, ["pT", "c32"], ["kdtok"])
                      fns = []
                      for h in range(8):
                          r, tl = h % 4, h // 4
                          fns.append(lambda g, h=h, r=r, tl=tl: g.matmul(Gb[r][0][:, tl * 128:(tl + 1) * 128], kb[32 * r:32 * r + 32, tl, :],
                                                                         qb[32 * r:32 * r + 32, tl, :], start=True, stop=True,
                                                                         tile_position=(32 * r, 0)))
                      A.group("pe", fns, ["kb", "qb"], ["pF0", "pF1", "pS", "pC"])
                      for r in range(4):
                          TT("dve", ST[:, r:8:4, :], Gb[r][0][:, 0:256].rearrange("p (a t) -> p a t", a=2), dmat[:, r:8:4, :], ALU.mult,
                             [Gb[r][1], "c32"], ["ST"])
                      pfy, pky = nextF()
                      fns = []
                      for h in range(8):
                          r, tl = h % 4, h // 4
                          po = pfy[64 * (h % 2):64 * (h % 2) + 64, (h // 2) * 128:(h // 2 + 1) * 128]
                          fns.append(lambda g, h=h, po=po: g.matmul(po, vtok[c][:, h * 64:(h + 1) * 64], ST[:, h, :], start=True, stop=False,
                                                                    tile_position=(0, 64 * (h % 2))))
                          fns.append(lambda g, h=h, po=po, r=r, tl=tl: g.matmul(po, rspad[:, h, :], qdb[:, tl, :],
                                                                                start=False, stop=True, tile_position=(0, 64 * (h % 2))))
                      A.group("pe", fns, [vk, "ST", "rspad", "qdb"], [pky])
                      pf, pk = nextF()
                      fns = []
                      for h in range(8):
                          r, tl = h % 4, h // 4
                          fns.append(lambda g, h=h, r=r, tl=tl: g.matmul(pf[32 * r:32 * r + 32, tl * 64:(tl + 1) * 64], kdtok[:, h * 32:(h + 1) * 32],
                                                                         vtok[c][:, h * 64:(h + 1) * 64], start=True, stop=True,
                                                                         tile_position=(0, 32 * r)))
                      A.group("pe", fns, ["kdtok", vk], [pk])
                      for tl in range(2):
                          STT(rstate[:, tl, :], rstate[:, tl, :], gch[:, tl:tl + 1], pf[:, tl * 64:(tl + 1) * 64], ALU.mult, ALU.add,
                              ["rstate", "c32", pk], ["rstate"])
                      for h in range(8):
                          TS("pool", rspad[:, h, :], rstate[:, h // 4, :], rmask[:, h % 4:h % 4 + 1], ALU.mult, ["rstate", "c32"], ["rspad"])
                      CP("dve", yr1[:].rearrange("p a t -> p (a t)"), pfy[:], [pky], ["yr1"])
                      ACT(yrsq[:], yr1[:], AF.Square, ["yr1"], ["yrsq"])
                      pf, pk = nextF()
                      A.group("pe", [(lambda g, i=i: g.matmul(pf[:, i * 128:(i + 1) * 128], blk64b, yrsq[:, i, :], start=True, stop=True))
                                     for i in range(4)], ["cbf", "yrsq"], [pk])
                      r4 = rstd4[:].rearrange("p a t -> p (a t)")
                      ACT(r4, pf[:], AF.Ln, [pk], ["rstd4"], bias=EPS, scale=1.0 / 64)
                      ACT(r4, r4, AF.Exp, ["rstd4"], ["rstd4"], scale=-0.5)
                      TT("dve", yr1[:], yr1[:], bc(ppc("retnw", 0, 4).unsqueeze(2), [P, 4, 128]), ALU.mult, ["yr1", "pp"], ["yr1"])
                      TT("dve", yr1[:], yr1[:], rstd4[:], ALU.mult, ["yr1", "rstd4"], ["yr1"])
                      TT("dve", yTret[:, :, cs_], yr1[:], rg[:, :, cs_], ALU.mult, ["yr1", "rg"], ["yTret"])
                      if debug and s == 0 and l == 0:
                          TT("dve", dbt[:, 0:4, :], yr1[:], rg[:, :, cs_], ALU.mult, ["yr1", "rg"], ["tB"])
                          A.dma("sp", "dbg", dbg["yret"][:, :, cs_], dbt[:, 0:4, :], reads=["tB"], writes=["dbg_out"])

                  stg(5)
                  acc = {(0, 0): (pC[:, 0:512], "pC"), (0, 1): (pC[:, 512:1024], "pC"), (1, 0): (pY[:, 0:512], "pY"), (1, 1): (pY[:, 512:1024], "pY")}
                  for go in range(8):
                      si = (NG + go) % NSLOT
                      wk = "ws%d" % si
                      wv = wslot[si][:].rearrange("p a b -> p (a b)").rearrange("p (a b) -> p a b", a=2)
                      A.dma("sp", wk, wv, woscr_d[go], reads=["woscr%d" % go], writes=[wk])
                      fns = []
                      for c in range(NCH):
                          cs_ = slice(c * T, (c + 1) * T)
                          for half in range(2):
                              for k2 in range(2):
                                  kt = 2 * go + k2
                                  if kt < 8:
                                      lt = yTssd[:, kt, cs_]
                                  elif kt < 12:
                                      lt = yTs5[:, kt - 8, cs_]
                                  else:
                                      lt = yTret[:, kt - 12, cs_]
                                  fns.append(lambda g, c=c, half=half, k2=k2, kt=kt, lt=lt: g.matmul(
                                      acc[(c, half)][0], lt, wv[:, k2, half * 512:(half + 1) * 512], start=(kt == 0), stop=(kt == 15)))
                      A.group("pe", fns, ["yTssd", "yTs5", "yTret", wk], ["pC", "pY"])
                  for c in range(NCH):
                      rows = slice(t0 + c * T, t0 + (c + 1) * T)
                      pacc, pkey = (pC, "pC") if c == 0 else (pY, "pY")
                      A.dma("sp", "hnld", hn[:], src_d[rows, :], writes=["hn"])
                      TT("dve", hn[:], hn[:], pacc[:], ALU.add, ["hn", pkey], ["hn"])
                      if not last:
                          A.dma("sp", "st", hres_d[rows, :], hn[:], reads=["hn"], writes=["hres"])
                      else:
                          ACT(xn[:], hn[:], AF.Square, ["hn"], ["xn", "small"], accum=small[:, 4:5])
                          ACT(small[:, 5:6], small[:, 4:5], AF.Ln, ["small"], ["small"], bias=EPS, scale=1.0 / DM)
                          ACT(small[:, 6:7], small[:, 5:6], AF.Exp, ["small"], ["small"], scale=-0.5)
                          STT(hn[:], hn[:], small[:, 6:7], fnw[:], ALU.mult, ALU.mult, ["hn", "small", "fnw"], ["hn"])
                          A.dma("sp", "st", out_d[rows, :], hn[:], reads=["hn"], writes=["outd"])
                  CP("dve", xbcT[:, :, 0:3], xbcT[:, :, SP:SP + 3], ["xbcT"], ["xbcT"])
              A.barrier()
              es3.close()
              open_stacks[:] = []

        except _Stop:
            for st_ in reversed(open_stacks):
                st_.close()
        A.finish("sp")
        build_program.last_nins = A.nins
    return nc


def make_consts():
    c32 = np.zeros((P, NC32), np.float64)
    k = np.arange(128)
    c32[:, C32["U"]:C32["U"] + 128] = (k[:, None] > k[None, :])
    c32[:, C32["ones"]:C32["ones"] + 128] = 1.0
    c32[:, C32["tri"]:C32["tri"] + 128] = (k[:, None] <= k[None, :])
    log_g = np.log1p(-np.exp2(-5.0 - np.arange(8)))
    scale = 32 ** -0.5
    diff = k[None, :] - k[:, None]
    for h in range(8):
        c32[:, C32["dmat"] + h * 128:C32["dmat"] + (h + 1) * 128] = np.where(diff >= 0, np.exp(np.maximum(diff, 0) * log_g[h]) * scale, 0.0)
    for tl in range(2):
        for h4 in range(4):
            h = 4 * tl + h4
            c32[32 * h4:32 * h4 + 32, C32["qdec"] + tl * 128:C32["qdec"] + (tl + 1) * 128] = np.exp((k + 1.0) * log_g[h])[None, :]
            c32[32 * h4:32 * h4 + 32, C32["gch"] + tl] = np.exp(128.0 * log_g[h])
    for h in range(8):
        c32[:, C32["kdec"] + h] = np.exp((127.0 - k) * log_g[h]) * scale
    inv_freq = 10000.0 ** (-np.arange(0, 32, 2) / 32.0)
    fr = inv_freq[np.arange(128) % 16]
    c32[:, C32["cos"]:C32["cos"] + 128] = np.cos(fr[:, None] * k[None, :])
    c32[:, C32["sin"]:C32["sin"] + 128] = np.sin(fr[:, None] * k[None, :])
    cidx = np.arange(64)
    c32[:, C32["cc"]:C32["cc"] + 64] = np.cos(fr[:, None] * 128.0 * cidx[None, :])
    c32[:, C32["sc"]:C32["sc"] + 64] = np.sin(fr[:, None] * 128.0 * cidx[None, :])
    for r in range(4):
        c32[32 * r:32 * r + 32, C32["rmask"] + r] = 1.0
    cbf = np.zeros((P, 512), np.float32)
    cbf[:, 0:128] = np.eye(128)
    cbf[:, 128:256] = 1.0
    perm = np.zeros((128, 128), np.float32)
    for m in range(128):
        if m % 32 < 16:
            perm[m + 16, m] = -1.0
        else:
            perm[m - 16, m] = 1.0
    cbf[:, 256:384] = perm
    blk = np.zeros((128, 128), np.float32)
    blk[0:64, 0:64] = 1.0
    blk[64:128, 64:128] = 1.0
    cbf[:, 384:512] = blk
    return c32.astype(np.float32), cbf


def prep_weights(inp):
    f = lambda a: np.asarray(a, dtype=np.float32)
    w_in = f(inp["w_in"])
    win_g = np.zeros((2, NG, P, 8, 256), np.float32)
    for l in range(2):
        wl = w_in[l].reshape(8, 128, 5136)
        for gi in range(18):
            for tt in range(2):
                c0 = FM_ORIG[2 * gi + tt]
                win_g[l, gi, :, :, tt * 128:(tt + 1) * 128] = wl[:, :, c0:c0 + 128].transpose(1, 0, 2)
        for hv in range(2):
            win_g[l, 18 + hv] = wl[:, :, 4112 + hv * 256:4112 + (hv + 1) * 256].transpose(1, 0, 2)
        win_g[l, 20, :, :, 0:16] = wl[:, :, 2560:2576].transpose(1, 0, 2)
    wout_h = f(inp["w_out"]).reshape(2, 16, 128, DM).transpose(0, 2, 1, 3).copy()
    wglu_h = f(inp["s5_w_glu"]).reshape(2, 4, 128, 512).transpose(0, 2, 1, 3).copy()
    pp = np.zeros((2, P, NPP), np.float32)
    pb = np.zeros((2, P, NPB), np.float32)
    for l in range(2):
        def put(name, arr):
            arr = np.asarray(arr, np.float32)
            if name in OFFB:
                pb[l, :, OFFB[name]:OFFB[name] + arr.shape[1]] = arr
            else:
                pp[l, :, OFF[name]:OFF[name] + arr.shape[1]] = arr
        put("normw", f(inp["norm_w"])[l].reshape(8, 128).T)
        cw = f(inp["conv_w"])[l]
        put("convw", cw.reshape(4, 12, 128).transpose(2, 1, 0).reshape(128, 48))
        put("convb", f(inp["conv_b"])[l].reshape(12, 128).T)
        put("dtb", np.broadcast_to(f(inp["dt_bias"])[l][None, :], (128, 16)))
        put("alog", np.broadcast_to(f(inp["a_log"])[l][None, :], (128, 16)))
        put("dssd", np.broadcast_to(f(inp["d_ssd"])[l][None, :], (128, 16)))
        put("ssdnw", f(inp["ssd_norm_w"])[l].reshape(8, 128).T)
        put("s5d", f(inp["s5_d"])[l].reshape(4, 128).T)
        put("bglu", f(inp["s5_b_glu"])[l].reshape(4, 128).T)
        put("retnw", f(inp["ret_norm_w"])[l].reshape(4, 128).T)
        lam_re = f(inp["s5_lambda_re"])[l].reshape(16, 2, 64)
        lam_im = f(inp["s5_lambda_im"])[l].reshape(16, 2, 64)
        put("lamre", lam_re.transpose(1, 2, 0).reshape(128, 16))
        put("lamim", lam_im.transpose(1, 2, 0).reshape(128, 16))
        ls = f(inp["s5_log_step"])[l].reshape(16, 2)
        put("lstep", np.broadcast_to(ls.T[:, None, :], (2, 64, 16)).reshape(128, 16))
        for nm, src, tr in (("bre", "s5_b_re", False), ("bim", "s5_b_im", False), ("cre", "s5_c_re", True), ("cim", "s5_c_im", True)):
            a = f(inp[src])[l]
            if tr:
                a = a.transpose(0, 2, 1)
            a = a.reshape(16, 2, 64, 16)
            o = np.zeros((2, 64, 16, 2, 16), np.float32)
            for par in range(2):
                o[par, :, :, par, :] = a[:, par].transpose(1, 0, 2)
            put(nm, o.reshape(128, 512))
    cbrow = f(inp["conv_b"])[:, None, :1280].copy()
    fnw = f(inp["final_norm_w"])[None, :].copy()
    c32, cbf = make_consts()
    return {"win_g": win_g, "wout_h": wout_h, "wglu_h": wglu_h, "pp": pp, "pb": pb, "cbrow": cbrow, "fnw": fnw,
            "cst32": c32, "cstbf": cbf}


_prog_cache = {}


def kernel(**inputs):
    x = np.asarray(inputs["x"], dtype=np.float32)
    B, Lx, _ = x.shape
    nspan = Lx // SP
    shared = prep_weights(inputs)
    key = (nspan, 2)
    if key not in _prog_cache:
        _prog_cache[key] = build_program(nspan=nspan, nlayer=2)
    nc = _prog_cache[key]
    ncores = 8
    in_maps = []
    for cidx in range(ncores):
        m = dict(shared)
        m["x"] = np.ascontiguousarray(x[cidx % B])
        in_maps.append(m)
    res = run_bass_kernel_spmd(nc, in_maps, core_ids=list(range(ncores)))
    out = np.stack([np.asarray(res.results[b]["out"], dtype=np.float32) for b in range(B)], axis=0)
    return out.astype(inputs["x"].dtype)
```
